# Optimizing a Trainium2 kernel written in Bass

```python
import jax, jax.numpy as jnp
from jax import lax
import numpy as np

D_MODEL = 1024
BATCH = 4
SEQ = 8192
DEPTH = 2

N_MIXERS = 2
D_FF = 2816
CONV_WIDTH = 3
MLSTM_HEADS = 4
MLSTM_QK_DIM = D_MODEL // 2 // MLSTM_HEADS
MLSTM_V_DIM = D_MODEL // MLSTM_HEADS
MLSTM_CHUNK = 64
MLSTM_IN_DIM = 2 * MLSTM_HEADS * MLSTM_QK_DIM + 2 * MLSTM_HEADS * MLSTM_V_DIM + 2 * MLSTM_HEADS
NORM_EPS = 1e-6

kernel_name = "hybrid_shortconv_mlstm_macaron"


def rms_norm(x, g):
    xf = x.astype(jnp.float32)
    y = xf * lax.rsqrt(jnp.mean(xf * xf, axis=-1, keepdims=True) + NORM_EPS)
    return (y * g.astype(jnp.float32)).astype(x.dtype)


def swiglu(x, w_gate, w_up, w_down):
    return (jax.nn.silu(x @ w_gate) * (x @ w_up)) @ w_down


def short_conv_mixer(x, w_in, conv_w, w_out):
    S = x.shape[1]
    gate_b, gate_c, h = jnp.split(x @ w_in, 3, axis=-1)
    u = gate_c * h
    up = jnp.pad(u, ((0, 0), (CONV_WIDTH - 1, 0), (0, 0)))
    conv = conv_w[0] * up[:, 0:S]
    for k in range(1, CONV_WIDTH):
        conv = conv + conv_w[k] * up[:, k:k + S]
    return (gate_b * conv) @ w_out


def mlstm_chunkwise(q, k, v, log_i, log_f):
    B, H, S, DQK = q.shape
    DV = v.shape[-1]
    L = MLSTM_CHUNK
    NC = S // L

    def to_chunks(t):
        return jnp.moveaxis(t.reshape(B, H, NC, L, *t.shape[3:]), 2, 0)

    causal = jnp.tril(jnp.ones((L, L), dtype=bool))

    def step(carry, inp):
        C, n, m = carry
        qj, kj, vj, ij, fj = inp
        bcum = jnp.cumsum(fj, axis=-1)
        log_d = bcum[..., :, None] - bcum[..., None, :] + ij[..., None, :]
        log_d = jnp.where(causal, log_d, -jnp.inf)
        log_inter = bcum + m[..., None]
        m_row = jnp.maximum(log_inter, jnp.max(log_d, axis=-1))
        w_intra = jnp.exp(log_d - m_row[..., None])
        w_inter = jnp.exp(log_inter - m_row)
        s = jnp.einsum('bhld,bhsd->bhls', qj, kj) * w_intra
        num = (jnp.einsum('bhls,bhsv->bhlv', s, vj)
               + w_inter[..., None] * jnp.einsum('bhld,bhdv->bhlv', qj, C))
        den = jnp.sum(s, axis=-1) + w_inter * jnp.einsum('bhld,bhd->bhl', qj, n)
        h = num / jnp.maximum(jnp.abs(den), jnp.exp(-m_row))[..., None]
        b_last = bcum[..., -1]
        log_w = b_last[..., None] - bcum + ij
        m_new = jnp.maximum(b_last + m, jnp.max(log_w, axis=-1))
        w_state = jnp.exp(log_w - m_new[..., None])
        decay = jnp.exp(b_last + m - m_new)
        C_new = decay[..., None, None] * C + jnp.einsum('bhs,bhsd,bhsv->bhdv', w_state, kj, vj)
        n_new = decay[..., None] * n + jnp.einsum('bhs,bhsd->bhd', w_state, kj)
        return (C_new, n_new, m_new), h

    init = (jnp.zeros((B, H, DQK, DV), jnp.float32),
            jnp.zeros((B, H, DQK), jnp.float32),
            jnp.zeros((B, H), jnp.float32))
    _, hc = lax.scan(step, init, tuple(to_chunks(t) for t in (q, k, v, log_i, log_f)))
    return jnp.moveaxis(hc, 0, 2).reshape(B, H, S, DV)


def mlstm_mixer(x, w_in, b_gates, head_norm, w_out):
    B, S, _ = x.shape
    NH, DQK, DV = MLSTM_HEADS, MLSTM_QK_DIM, MLSTM_V_DIM
    splits = [NH * DQK, 2 * NH * DQK, 2 * NH * DQK + NH * DV, 2 * NH * DQK + 2 * NH * DV]
    q, k, v, o, g = jnp.split(x @ w_in, splits, axis=-1)

    def heads(t, d):
        return t.reshape(B, S, NH, d).transpose(0, 2, 1, 3).astype(jnp.float32)

    q = heads(q, DQK)
    k = heads(k, DQK) * (DQK ** -0.5)
    v = heads(v, DV)
    g = (g.astype(jnp.float32) + b_gates.astype(jnp.float32)).transpose(0, 2, 1)
    log_i = g[:, :NH]
    log_f = jax.nn.log_sigmoid(g[:, NH:])
    h = mlstm_chunkwise(q, k, v, log_i, log_f)
    h = h * lax.rsqrt(jnp.mean(h * h, axis=-1, keepdims=True) + NORM_EPS)
    h = h * head_norm.astype(jnp.float32)[None, :, None, :]
    h = h.transpose(0, 2, 1, 3).reshape(B, S, NH * DV).astype(x.dtype)
    return (h * jax.nn.sigmoid(o)) @ w_out


def setup_inputs(seed: int = 0) -> dict:
    key = jax.random.key(seed)
    ks = jax.random.split(key, 20)
    D, F = D_MODEL, D_FF
    n_conv = (DEPTH + 1) // 2
    n_ml = DEPTH // 2
    nrm = lambda k, shape, scale: jax.random.normal(k, shape, jnp.float32) * scale
    forget_bias = jnp.linspace(3.0, 6.0, MLSTM_HEADS, dtype=jnp.float32)
    b_gates = jnp.concatenate([
        nrm(ks[10], (n_ml, MLSTM_HEADS), 0.1),
        forget_bias[None, :] + nrm(ks[11], (n_ml, MLSTM_HEADS), 0.1)], axis=-1)
    return {
        "x": nrm(ks[0], (BATCH, SEQ, D), 1.0),
        "norm_g": 1.0 + nrm(ks[1], (DEPTH, 3, D), 0.02),
        "ffn_w_gate": nrm(ks[2], (DEPTH, 2, D, F), D ** -0.5),
        "ffn_w_up": nrm(ks[3], (DEPTH, 2, D, F), D ** -0.5),
        "ffn_w_down": nrm(ks[4], (DEPTH, 2, F, D), F ** -0.5),
        "conv_w_in": nrm(ks[5], (n_conv, D, 3 * D), D ** -0.5),
        "conv_w": nrm(ks[6], (n_conv, CONV_WIDTH, D), CONV_WIDTH ** -0.5),
        "conv_w_out": nrm(ks[7], (n_conv, D, D), D ** -0.5),
        "mlstm_w_in": nrm(ks[8], (n_ml, D, MLSTM_IN_DIM), D ** -0.5),
        "mlstm_b_gates": b_gates,
        "mlstm_head_norm": 1.0 + nrm(ks[12], (n_ml, MLSTM_HEADS, MLSTM_V_DIM), 0.02),
        "mlstm_w_out": nrm(ks[9], (n_ml, D, D), D ** -0.5),
        "final_norm_g": 1.0 + nrm(ks[13], (D,), 0.02),
    }


def reference(x, norm_g, ffn_w_gate, ffn_w_up, ffn_w_down, conv_w_in, conv_w, conv_w_out,
              mlstm_w_in, mlstm_b_gates, mlstm_head_norm, mlstm_w_out, final_norm_g):
    h = x
    for layer in range(DEPTH):
        h = h + 0.5 * swiglu(rms_norm(h, norm_g[layer, 0]),
                             ffn_w_gate[layer, 0], ffn_w_up[layer, 0], ffn_w_down[layer, 0])
        hn = rms_norm(h, norm_g[layer, 1])
        j = layer // N_MIXERS
        if layer % N_MIXERS == 0:
            mix = short_conv_mixer(hn, conv_w_in[j], conv_w[j], conv_w_out[j])
        else:
            mix = mlstm_mixer(hn, mlstm_w_in[j], mlstm_b_gates[j], mlstm_head_norm[j], mlstm_w_out[j])
        h = h + mix
        h = h + 0.5 * swiglu(rms_norm(h, norm_g[layer, 2]),
                             ffn_w_gate[layer, 1], ffn_w_up[layer, 1], ffn_w_down[layer, 1])
    return rms_norm(h, final_norm_g)
```

```python
import numpy as np
import ml_dtypes
from contextlib import ExitStack
import concourse.bass as bass
import concourse.mybir as mybir
from concourse.bass_utils import run_bass_kernel_spmd

F32 = mybir.dt.float32
BF16 = mybir.dt.bfloat16
ALU = mybir.AluOpType
AF = mybir.ActivationFunctionType

NCORES = 8
D = 1024
KC = 8
DFF = 2816
SEQ = 8192
TOK = 4096
TILE = 1024
HALO = 2
TC = TILE + HALO
NT = TOK // TILE
SUB = 512
NH = 4
DQK = 128
DV = 256
DVE_ = DV + 1
MIN_DIM = 3080
EPS = 1e-6
LN_KSCALE = float(np.log(DQK ** -0.5))

O_ID = 0
O_TRI = 128
O_G = 256
O_GF = 304
O_CW = 312
O_HN = 336
O_BG = 344
O_MASK = 352
NCONST = 356

WSLOT = 5632
NWS = 4


class Res:
    __slots__ = ("name", "w", "r", "excl")

    def __init__(self, name, excl=False):
        self.name = name
        self.w = None
        self.r = []
        self.excl = excl


class DmaSem:
    __slots__ = ("name", "count", "sem", "inc")

    def __init__(self, name, inc=16):
        self.name = name
        self.count = 0
        self.sem = None
        self.inc = inc


class Op:
    __slots__ = ("eng", "fn", "deps", "idx", "dma", "ndma", "needs_inc", "val")


class Prog:
    ENGS = ("pe", "act", "dve", "pool", "sp")

    def __init__(self):
        self.ops = []
        self.dmasems = []

    def dmasem(self, name, inc=16):
        for s in self.dmasems:
            if s.name == name:
                return s
        s = DmaSem(name, inc)
        self.dmasems.append(s)
        return s

    def add(self, eng, fn, reads=(), writes=(), dma=None, ndma=1):
        op = Op()
        op.eng = eng
        op.fn = fn
        op.idx = len(self.ops)
        op.dma = dma
        op.ndma = ndma
        op.needs_inc = False
        op.val = 0
        deps = set()
        for r in reads:
            if r.w is not None:
                deps.add(r.w)
            if r.excl:
                deps.update(x for x in r.r if self.ops[x].eng != eng)
        for r in writes:
            if r.w is not None:
                deps.add(r.w)
            deps.update(r.r)
        for r in reads:
            r.r.append(op.idx)
        for r in writes:
            r.w = op.idx
            r.r = []
        deps.discard(op.idx)
        if eng == "pe" and dma is None:
            deps = {d for d in deps if not (self.ops[d].eng == "pe" and self.ops[d].dma is None)}
        op.deps = deps
        self.ops.append(op)
        return op

    def emit(self, nc, es):
        ops = self.ops
        for op in ops:
            for d in op.deps:
                ops[d].needs_inc = True
        cnt = {e: 0 for e in self.ENGS}
        for op in ops:
            if op.dma is not None:
                op.dma.count += op.dma.inc * op.ndma
                op.val = op.dma.count
            elif op.needs_inc:
                cnt[op.eng] += 1
                op.val = cnt[op.eng]
        esem = {e: es.enter_context(nc.semaphore("s_" + e)) for e in self.ENGS}
        for s in self.dmasems:
            if s.count > 0:
                s.sem = es.enter_context(nc.semaphore("d_" + s.name))
        block = es.enter_context(nc.Block())
        starters = {"pe": block.tensor, "act": block.scalar, "dve": block.vector,
                    "pool": block.gpsimd, "sp": block.sync}
        for e in self.ENGS:
            eops = [op for op in ops if op.eng == e]
            if not eops:
                continue

            def body(eng, eops=eops, e=e):
                waited = {}
                for op in eops:
                    need = {}
                    for d in op.deps:
                        dop = ops[d]
                        if dop.dma is not None:
                            key = ("d", dop.dma.name)
                            sem = dop.dma.sem
                        else:
                            key = ("e", dop.eng)
                            sem = esem[dop.eng]
                        if need.get(key, (None, 0))[1] < dop.val:
                            need[key] = (sem, dop.val)
                    for key, (sem, val) in need.items():
                        if waited.get(key, 0) < val:
                            eng.wait_ge(sem, val)
                            waited[key] = val
                    res = op.fn(eng)
                    if op.dma is not None:
                        insts = res if isinstance(res, (list, tuple)) else [res]
                        assert len(insts) == op.ndma, (len(insts), op.ndma)
                        for i_ in insts:
                            if op.dma.inc == 16:
                                i_.then_inc(op.dma.sem, 16)
                            else:
                                i_.then_inc(op.dma.sem)
                    elif op.needs_inc:
                        inst = res[-1] if isinstance(res, (list, tuple)) else res
                        inst.then_inc(esem[e], 1)

            starters[e](body)


def build(mode):
    do1 = mode in ("p1", "fused")
    do2 = mode in ("p2", "fused")
    nc = bass.Bass("TRN2", target_bir_lowering=False)
    P = Prog()
    es = ExitStack()

    def dram(name, shape, dt, kind):
        return nc.dram_tensor(name, list(shape), dt, kind=kind).ap()

    consts_d = dram("consts", [128, NCONST], F32, "ExternalInput")
    wgate_d = dram("ffn_w_gate", [2, 2, D, DFF], F32, "ExternalInput")
    wup_d = dram("ffn_w_up", [2, 2, D, DFF], F32, "ExternalInput")
    wdown_d = dram("ffn_w_down", [2, 2, DFF, D], F32, "ExternalInput")
    if do1:
        x_d = dram("x", [TOK + HALO, D], F32, "ExternalInput")
        cwin_d = dram("conv_w_in", [D, 3 * D], F32, "ExternalInput")
        cwout_d = dram("conv_w_out", [D, D], F32, "ExternalInput")
        mwin_d = dram("mlstm_w_in", [D, MIN_DIM], F32, "ExternalInput")
    if do2:
        mwout_d = dram("mlstm_w_out", [D, D], F32, "ExternalInput")
        out_d = dram("out", [TOK, D], F32, "ExternalOutput")
    k1 = "ExternalOutput" if mode == "p1" else ("ExternalInput" if mode == "p2" else "Internal")
    sp_h = dram("sp_h", [NT, 128, KC * TILE], F32, k1)
    sp_q = dram("sp_q", [NT, 128, NH * TILE], BF16, k1)
    sp_k = dram("sp_k", [NT, 128, NH * TILE], BF16, k1)
    sp_o = dram("sp_o", [NT, 128, KC * TILE], BF16, k1)
    sp_kc = dram("sp_kc", [NT * 8, 128, NH * DQK], BF16, k1)
    sp_v = dram("sp_v", [NT * 8, 128, NH * DVE_], BF16, k1)
    sp_g = dram("sp_g", [NT, 128, 8 * 16], F32, k1)
    if mode == "p1":
        st_out = dram("st_out", [128, NH * DVE_], F32, "ExternalOutput")
    if mode == "p2":
        st_in = dram("st_in", [128, NH * DVE_], F32, "ExternalInput")
    if mode == "fused":
        st_loc = dram("st_loc", [128, NH * DVE_], F32, "Internal")
        st_all = dram("st_all", [256, NH * DVE_], F32, "Internal")

    def sb(name, shape, dt):
        return es.enter_context(nc.sbuf_tensor(name, list(shape), dt))

    cst = sb("cst", [128, NCONST], F32)
    ident_bf = sb("ident_bf", [128, 128], BF16)
    ones_bf = sb("ones_bf", [128, 128], BF16)
    ones32 = sb("ones32", [128, 128], F32)
    h = sb("h", [128, KC, TC], F32)
    xn = sb("xn", [128, KC, TC], BF16)
    hid = sb("hid", [128, 11, TC], BF16)
    wr = [sb("wr%d" % i, [128, WSLOT], BF16) for i in range(NWS)]
    sq = sb("sq", [128, KC, SUB], BF16)
    rstd = sb("rstd", [128, TC], F32)
    lnt = sb("lnt", [128, SUB], F32)
    gact = [sb("gact%d" % i, [128, SUB], F32) for i in range(2)]
    xst = [sb("xst%d" % i, [128, D], F32) for i in range(4)]
    gq = sb("gq", [128, 8, 16], F32)
    C32 = sb("C32", [128, NH, DVE_], F32)
    sm = sb("sm", [128, 64], F32)
    if do1:
        gbs = sb("gbs", [128, TC], F32)
        acc = sb("acc", [128, TILE], F32)
        ucarry = sb("ucarry", [128, KC, 2], F32)
    if do2:
        kc2c = [sb("kc2c%d" % i, [128, NH * DQK], BF16) for i in range(3)]
        vc = [sb("vc%d" % i, [128, NH, DVE_], BF16) for i in range(3)]
        soc = [sb("soc%d" % i, [128, KC, 128], BF16) for i in range(3)]
        Cbf = sb("Cbf", [128, NH, DVE_], BF16)
        PT = [sb("PT%d" % i, [128, 128], BF16) for i in range(2)]
        hnb = [sb("hnb%d" % i, [128, DV], BF16) for i in range(4)]

    psb = [es.enter_context(nc.psum_tensor("ps%d" % i, [128, 512], F32)) for i in range(6)]
    pstb = [es.enter_context(nc.psum_tensor("pst%d" % i, [128, 1024], BF16)) for i in range(2)]

    R = {}

    def res(name):
        if name not in R:
            R[name] = Res(name)
        return R[name]

    for i_ in range(6):
        R["ps%d" % i_] = Res("ps%d" % i_, excl=True)
    for i_ in range(2):
        R["pst%d" % i_] = Res("pst%d" % i_, excl=True)
    r_ps = [res("ps%d" % i) for i in range(6)]
    r_pstb = [res("pst0"), res("pst1")]
    r_cst = res("cst")
    r_wr = [res("wr%d" % i) for i in range(NWS)]
    s_wr = [P.dmasem("wr%d" % i) for i in range(NWS)]

    SUBS = [(HALO, SUB), (HALO + SUB, SUB)]
    HALO_SUB = (0, HALO)

    def rH(kc, s):
        return res("h_%d_%d" % (kc, s))

    def rX(kc, s):
        return res("xn_%d_%d" % (kc, s))

    def rHid(j, s):
        return res("hid_%d_%d" % (j, s))

    mmrot = [0]

    def mmbank():
        i = mmrot[0] % 6
        mmrot[0] += 1
        return psb[i], r_ps[i]


    wcount = [0]

    def wload(pieces, kc):
        i = wcount[0] % NWS
        wcount[0] += 1
        offs = []
        o = 0
        for ap_ in pieces:
            offs.append(o)
            o += ap_.shape[2]
        tot = o
        assert kc * tot <= WSLOT

        def fn(eng, i=i, pieces=pieces, offs=offs, tot=tot, kc=kc):
            insts = []
            dst = wr[i][:, 0:kc * tot].rearrange("p (k n) -> p k n", k=kc)
            for ap_, o_ in zip(pieces, offs):
                insts.append(eng.dma_start(out=dst[:, :, o_:o_ + ap_.shape[2]], in_=ap_))
            return insts

        P.add("pool", fn, reads=(), writes=(r_wr[i],), dma=s_wr[i], ndma=len(pieces))
        view = wr[i][:, 0:kc * tot].rearrange("p (k n) -> p k n", k=kc)
        return view, r_wr[i], offs

    def wview(wd, c0, cw):
        return wd.rearrange("(k p) n -> p k n", p=128)[:, :, c0:c0 + cw]

    s_cst = P.dmasem("cst")
    P.add("sp", lambda eng: eng.dma_start(out=cst[:, :], in_=consts_d[:, :]),
          writes=(r_cst,), dma=s_cst)
    r_cbf = res("cbf")
    P.add("dve", lambda eng: eng.tensor_copy(out=ident_bf[:, :], in_=cst[:, O_ID:O_ID + 128]),
          reads=(r_cst,), writes=(r_cbf,))
    P.add("dve", lambda eng: eng.memset(ones_bf[:, :], 1.0), writes=(r_cbf,))
    P.add("dve", lambda eng: eng.memset(ones32[:, :], 1.0), writes=(r_cbf,))
    ident32 = cst[:, O_ID:O_ID + 128]
    tri32 = cst[:, O_TRI:O_TRI + 128]

    carry = []

    def flush_carry():
        while carry:
            carry.pop(0)()

    def norm_parts(gcol, si, c0, n, out_bf=True):
        r_sq = res("sq")
        r_lnt = res("lnt")
        r_rs = res("rstd_%d" % si)

        def partA():
            P.add("act", lambda eng: eng.activation(out=sq[:, :, 0:n], in_=h[:, :, c0:c0 + n], func=AF.Square),
                  reads=[rH(k, si) for k in range(KC)], writes=(r_sq,))

        def partB():
            AUX, r_AUX = mmbank()

            def f_mm(eng):
                out = None
                for k in range(KC):
                    out = eng.matmul(AUX[:, 0:n], lhsT=ones_bf[:, :], rhs=sq[:, k, 0:n],
                                     start=(k == 0), stop=(k == KC - 1))
                return out

            P.add("pe", f_mm, reads=(r_sq, r_cbf), writes=(r_AUX,))
            P.add("act", lambda eng: eng.activation(out=lnt[:, 0:n], in_=AUX[:, 0:n], func=AF.Ln,
                                                    bias=float(EPS), scale=1.0 / D),
                  reads=(r_AUX,), writes=(r_lnt,))
            P.add("act", lambda eng: eng.activation(out=rstd[:, c0:c0 + n], in_=lnt[:, 0:n], func=AF.Exp,
                                                    scale=-0.5),
                  reads=(r_lnt,), writes=(r_rs,))
            for kc in range(KC):
                dst = xn if out_bf else h
                r_dst = rX(kc, si) if out_bf else rH(kc, si)
                P.add("dve", lambda eng, kc=kc, dst=dst: eng.scalar_tensor_tensor(
                    out=dst[:, kc, c0:c0 + n], in0=h[:, kc, c0:c0 + n],
                    scalar=cst[:, gcol + kc:gcol + kc + 1], in1=rstd[:, c0:c0 + n],
                    op0=ALU.mult, op1=ALU.mult),
                    reads=(rH(kc, si), r_rs, r_cst), writes=(r_dst,))

        return partA, partB

    def rmsnorm(gcol, subs, out_bf=True):
        for si, (c0, n) in subs:
            a, b = norm_parts(gcol, si, c0, n, out_bf)
            a()
            b()

    def final_update(blocks, kcn, rhs_fn, rhs_res, evac, subs, nxt):
        groups = [(mb, mm) for mb in range(len(blocks)) for mm in range(4)]

        def one(si, s0, n, mb, mm):
            wv, rw = blocks[mb]
            m = mb * 4 + mm
            pd, rpd = mmbank()
            mm_group(pd, rpd, wv, rw, mm * 128, kcn, lambda k: rhs_fn(k, s0, n), rhs_res(si), n)
            evac(pd, rpd, m, si, s0, n)

        if nxt is None or len(subs) != 2:
            for (mb, mm) in groups:
                for si, (s0, n) in subs:
                    one(si, s0, n, mb, mm)
            return
        (sa, (a0, an)), (sb_, (b0, bn)) = subs
        A0, B0 = norm_parts(nxt[0], sa, a0, an, nxt[1])
        A1, B1 = norm_parts(nxt[0], sb_, b0, bn, nxt[1])
        for (mb, mm) in groups:
            one(sa, a0, an, mb, mm)
        A0()
        for gi, (mb, mm) in enumerate(groups):
            one(sb_, b0, bn, mb, mm)
            if gi == 2:
                B0()
        A1()
        carry.append(B1)

    def mm_group(ps, r_p, wv, r_w, wcol, kcn, rhs_fn, rhs_res, n, mcols=128):
        def fn(eng):
            out = None
            for k in range(kcn):
                out = eng.matmul(ps[0:mcols, 0:n], lhsT=wv[:, k, wcol:wcol + mcols], rhs=rhs_fn(k),
                                 start=(k == 0), stop=(k == kcn - 1))
            return out

        P.add("pe", fn, reads=[r_w] + list(rhs_res), writes=(r_p,))

    def ffn(l, i, subs, pre=False, nxt=None):
        gcol = O_G + (l * 3 + (0 if i == 0 else 2)) * 8
        if not pre:
            rmsnorm(gcol, subs)
        gbuf = [0]
        r_ga = [res("gact0"), res("gact1")]
        for hh in range(2):
            j0 = hh * 11
            for (jb, nj) in ((0, 4), (4, 4), (8, 3)):
                c0 = (j0 + jb) * 128
                wg, rg, _ = wload([wview(wgate_d[l, i], c0, nj * 128)], KC)
                wu, ru, _ = wload([wview(wup_d[l, i], c0, nj * 128)], KC)
                for si, (s0, n) in subs:
                    for jj in range(nj):
                        j = jb + jj
                        pg, rpg = mmbank()
                        pu, rpu = mmbank()
                        xr = [rX(k, si) for k in range(KC)]
                        mm_group(pg, rpg, wg, rg, jj * 128, KC,
                                 lambda k, s0=s0, n=n: xn[:, k, s0:s0 + n], xr, n)
                        mm_group(pu, rpu, wu, ru, jj * 128, KC,
                                 lambda k, s0=s0, n=n: xn[:, k, s0:s0 + n], xr, n)
                        b = gbuf[0] % 2
                        gbuf[0] += 1
                        P.add("act", lambda eng, pg=pg, n=n, b=b: eng.activation(
                            out=gact[b][:, 0:n], in_=pg[:, 0:n], func=AF.Silu),
                            reads=(rpg,), writes=(r_ga[b],))
                        P.add("dve", lambda eng, pu=pu, n=n, b=b, j=j, s0=s0: eng.tensor_tensor(
                            out=hid[:, j, s0:s0 + n], in0=pu[:, 0:n], in1=gact[b][:, 0:n],
                            op=ALU.mult),
                            reads=(rpu, r_ga[b]), writes=(rHid(j, si),))
                        if jj == 1:
                            flush_carry()
            def evac_down(pd, rpd, m, si, s0, n):
                P.add("dve", lambda eng: eng.scalar_tensor_tensor(
                    out=h[:, m, s0:s0 + n], in0=pd[:, 0:n], scalar=0.5,
                    in1=h[:, m, s0:s0 + n], op0=ALU.mult, op1=ALU.add),
                    reads=(rpd, rH(m, si)), writes=(rH(m, si),))

            blocks = []
            for mb in range(2):
                wdv, rd, _ = wload([wdown_d[l, i][j0 * 128:(j0 + 11) * 128, :].rearrange(
                    "(k p) n -> p k n", p=128)[:, :, mb * 512:(mb + 1) * 512]], 11)
                blocks.append((wdv, rd))
            final_update(blocks, 11, lambda k, s0, n: hid[:, k, s0:s0 + n],
                         lambda si: [rHid(k, si) for k in range(11)], evac_down, subs,
                         nxt if hh == 1 else None)

    def out_proj(wd, subs, nxt=None):
        def evac_add(pd, rpd, m, si, s0, n):
            P.add("dve", lambda eng: eng.tensor_tensor(
                out=h[:, m, s0:s0 + n], in0=pd[:, 0:n], in1=h[:, m, s0:s0 + n], op=ALU.add),
                reads=(rpd, rH(m, si)), writes=(rH(m, si),))

        blocks = []
        for mb in range(2):
            wv, rw, _ = wload([wview(wd, mb * 512, 512)], KC)
            blocks.append((wv, rw))
        final_update(blocks, KC, lambda k, s0, n: hid[:, k, s0:s0 + n],
                     lambda si: [rHid(k, si) for k in range(KC)], evac_add, subs, nxt)

    r_xst = [res("xst%d" % i) for i in range(4)]
    s_xst = [P.dmasem("xst%d" % i) for i in range(4)]
    xcnt = [0]

    def load_x(t):
        for tb in range(8):
            load_x_tb(t, tb)
        if t == 0:
            load_x_halo()

    def load_x_tb(t, tb):
        if True:
            b = xcnt[0] % 4
            xcnt[0] += 1
            row0 = HALO + t * TILE + tb * 128
            P.add("sp", lambda eng, b=b, row0=row0: eng.dma_start(
                out=xst[b][:, :], in_=x_d[row0:row0 + 128, :]), writes=(r_xst[b],), dma=s_xst[b])
            si = tb // 4
            c0 = HALO + tb * 128
            for g in range(2):
                pT, rpT = mmbank()

                def f_tr(eng, b=b, g=g, pT=pT):
                    out = None
                    for q in range(4):
                        kc = g * 4 + q
                        out = eng.transpose(out=pT[:, q * 128:(q + 1) * 128],
                                            in_=xst[b][:, kc * 128:(kc + 1) * 128],
                                            identity=ident32)
                    return out

                P.add("pe", f_tr, reads=(r_xst[b], r_cst), writes=(rpT,))
                P.add("act" if g == 0 else "dve", lambda eng, g=g, c0=c0, pT=pT: (
                    eng.activation(out=h[:, g * 4:g * 4 + 4, c0:c0 + 128],
                                   in_=pT[:, :].rearrange("p (a b) -> p a b", a=4), func=AF.Copy)
                    if g == 0 else
                    eng.tensor_copy(out=h[:, g * 4:g * 4 + 4, c0:c0 + 128],
                                    in_=pT[:, :].rearrange("p (a b) -> p a b", a=4))),
                    reads=(rpT,), writes=[rH(g * 4 + q, si) for q in range(4)])
    def load_x_halo():
        if True:
            b = xcnt[0] % 4
            xcnt[0] += 1
            P.add("sp", lambda eng, b=b: eng.dma_start(
                out=xst[b][0:HALO, :], in_=x_d[0:HALO, :]), writes=(r_xst[b],), dma=s_xst[b])

            AUX, r_AUX = mmbank()

            def f_trh(eng, b=b):
                out = None
                for kc in range(KC):
                    out = eng.transpose(out=AUX[:, kc * HALO:(kc + 1) * HALO],
                                        in_=xst[b][0:HALO, kc * 128:(kc + 1) * 128],
                                        identity=cst[0:HALO, O_ID:O_ID + HALO])
                return out

            P.add("pe", f_trh, reads=(r_xst[b], r_cst), writes=(r_AUX,))
            P.add("dve", lambda eng: eng.tensor_copy(
                out=h[:, :, 0:HALO],
                in_=AUX[:, 0:KC * HALO].rearrange("p (a b) -> p a b", a=KC)),
                reads=(r_AUX,), writes=[rH(k, 2) for k in range(KC)])

    def conv_mixer(t, subs, pre=False, nxt=None):
        if not pre:
            rmsnorm(O_G + 1 * 8, subs)
        r_gc = [res("gact0"), res("gact1")]
        r_u = res("u")
        r_gb = res("gbs")
        r_acc = res("acc")
        r_uc = res("ucarry")
        u = rstd
        r_rs_all = [res("rstd_%d" % si) for si in range(3)]
        gbuf = [0]
        for cg in range(2):
            wvb, rwb, _ = wload([wview(cwin_d, 0 * D + cg * 512, 512)], KC)
            wvc, rwc, _ = wload([wview(cwin_d, 1 * D + cg * 512, 512)], KC)
            wvh, rwh, _ = wload([wview(cwin_d, 2 * D + cg * 512, 512)], KC)
            for cc in range(4):
                c = cg * 4 + cc
                if t > 0:
                    P.add("dve", lambda eng, c=c: eng.tensor_copy(out=u[:, 0:HALO], in_=ucarry[:, c, :]),
                          reads=(r_uc,), writes=[r_u] + r_rs_all)
                for si, (s0, n) in subs:
                    xr = [rX(k, si) for k in range(KC)]
                    rhs = lambda k, s0=s0, n=n: xn[:, k, s0:s0 + n]
                    pc, rpc = mmbank()
                    ph, rph = mmbank()
                    pb, rpb = mmbank()
                    mm_group(pc, rpc, wvc, rwc, cc * 128, KC, rhs, xr, n)
                    mm_group(ph, rph, wvh, rwh, cc * 128, KC, rhs, xr, n)
                    mm_group(pb, rpb, wvb, rwb, cc * 128, KC, rhs, xr, n)
                    b = gbuf[0] % 2
                    gbuf[0] += 1
                    P.add("act", lambda eng, pc=pc, n=n, b=b: eng.activation(
                        out=gact[b][:, 0:n], in_=pc[:, 0:n], func=AF.Copy),
                        reads=(rpc,), writes=(r_gc[b],))
                    P.add("dve", lambda eng, ph=ph, n=n, b=b, s0=s0: eng.tensor_tensor(
                        out=u[:, s0:s0 + n], in0=ph[:, 0:n], in1=gact[b][:, 0:n], op=ALU.mult),
                        reads=(rph, r_gc[b]), writes=[r_u] + r_rs_all)
                    P.add("act", lambda eng, pb=pb, n=n, s0=s0: eng.activation(
                        out=gbs[:, s0:s0 + n], in_=pb[:, 0:n], func=AF.Copy),
                        reads=(rpb,), writes=(r_gb,))
                    flush_carry()
                P.add("dve", lambda eng, c=c: eng.tensor_scalar(
                    out=acc[:, :], in0=u[:, 0:TILE], scalar1=cst[:, O_CW + c:O_CW + c + 1],
                    scalar2=None, op0=ALU.mult),
                    reads=(r_u, r_cst), writes=(r_acc,))
                P.add("dve", lambda eng, c=c: eng.scalar_tensor_tensor(
                    out=acc[:, :], in0=u[:, 1:TILE + 1], scalar=cst[:, O_CW + 8 + c:O_CW + 8 + c + 1],
                    in1=acc[:, :], op0=ALU.mult, op1=ALU.add),
                    reads=(r_u, r_cst, r_acc), writes=(r_acc,))
                P.add("dve", lambda eng, c=c: eng.scalar_tensor_tensor(
                    out=acc[:, :], in0=u[:, 2:TILE + 2], scalar=cst[:, O_CW + 16 + c:O_CW + 16 + c + 1],
                    in1=acc[:, :], op0=ALU.mult, op1=ALU.add),
                    reads=(r_u, r_cst, r_acc), writes=(r_acc,))
                P.add("dve", lambda eng, c=c: eng.tensor_copy(out=ucarry[:, c, :], in_=u[:, TILE:TILE + HALO]),
                      reads=(r_u,), writes=(r_uc,))
                P.add("dve", lambda eng, c=c: eng.tensor_tensor(
                    out=hid[:, c, HALO:HALO + TILE], in0=acc[:, :], in1=gbs[:, HALO:HALO + TILE],
                    op=ALU.mult),
                    reads=(r_acc, r_gb), writes=[rHid(c, 0), rHid(c, 1)])
        out_proj(cwout_d, [(0, SUBS[0]), (1, SUBS[1])], nxt)

    def mlstm_inproj(t, pre=False, hook=None):
        subs = [(0, SUBS[0]), (1, SUBS[1])]
        if pre:
            flush_carry()
        else:
            rmsnorm(O_G + (3 + 1) * 8, subs)
        spill_h(t)
        r_hid_all = [rHid(j, s) for j in range(11) for s in range(2)]
        kc2all = lambda tb: hid[:, tb // 2, (tb % 2) * 512:(tb % 2) * 512 + 512]
        fmst = [hid[:, 4, 0:TILE], hid[:, 5, 0:TILE]]
        vst = [hid[:, 6, 0:2 * DVE_].rearrange("p (a b) -> p a b", a=2),
               hid[:, 7, 0:2 * DVE_].rearrange("p (a b) -> p a b", a=2)]
        r_kc2 = [res("kc2all_%d" % tb) for tb in range(8)]
        r_fm = [res("fmst0"), res("fmst1")]
        s_fm = [P.dmasem("fmst0"), P.dmasem("fmst1")]
        r_vs = [res("vst0"), res("vst1")]
        s_vs = [P.dmasem("vst0"), P.dmasem("vst1")]
        s_kc = P.dmasem("kc2st")
        r_gq = res("gq")
        s_gq = P.dmasem("gq")
        r_sm = res("sm")
        r_C = [res("C32_%d" % hh) for hh in range(NH)]
        P.add("dve", lambda eng: eng.memset(vst[0][:, :, DV:DVE_], 1.0),
              writes=r_hid_all + r_vs + r_fm + r_kc2)
        P.add("dve", lambda eng: eng.memset(vst[1][:, :, DV:DVE_], 1.0), writes=[r_vs[1]])
        fcnt = [0]

        def fm_proj(c0, ncol, dst, func, hook=None):
            for blk in range(ncol // 512):
                wv, rw, _ = wload([wview(mwin_d, c0 + blk * 512, 512)], KC)
                for mm in range(4):
                    m = blk * 4 + mm
                    b = fcnt[0] % 2
                    fcnt[0] += 1
                    for si, (s0, n) in subs:
                        pp, rpp = mmbank()
                        mm_group(pp, rpp, wv, rw, mm * 128, KC,
                                 lambda k, s0=s0, n=n: xn[:, k, s0:s0 + n],
                                 [rX(k, si) for k in range(KC)], n)
                        P.add("act", lambda eng, pp=pp, n=n, b=b, si=si: eng.activation(
                            out=fmst[b][:, si * SUB:si * SUB + n], in_=pp[:, 0:n], func=func),
                            reads=(rpp,), writes=(r_fm[b],))
                    P.add("sp", lambda eng, b=b, m=m: eng.dma_start(
                        out=dst[t][:, m * TILE:(m + 1) * TILE], in_=fmst[b]),
                        reads=(r_fm[b],), writes=(res("dram_%d" % t),), dma=s_fm[b])
                    if hook is not None:
                        hook(m)

        fm_proj(0, 512, sp_q, AF.Copy)
        fm_proj(512, 512, sp_k, AF.Copy)
        fm_proj(2048, 1024, sp_o, AF.Sigmoid, hook)

        wv_, rw, _ = wload([wview(mwin_d, MIN_DIM - 128, 128)], KC)
        wv = wv_[:, :, 120:128]
        G8, E1, SP4, T4, T4B = (sm[:, 0:8], sm[:, 8:12], sm[:, 12:16], sm[:, 16:20], sm[:, 20:24])
        for tb in range(8):
            si = tb // 4
            tc0 = HALO + tb * 128
            pg, rpg = mmbank()

            def f_g(eng, pg=pg, tc0=tc0, wv=wv):
                out = None
                for k in range(KC):
                    out = eng.matmul(pg[:, 0:8], lhsT=xn[:, k, tc0:tc0 + 128], rhs=wv[:, k, 0:8],
                                     start=(k == 0), stop=(k == KC - 1))
                return out

            P.add("pe", f_g, reads=[rw] + [rX(k, si) for k in range(KC)], writes=(rpg,))
            P.add("dve", lambda eng, pg=pg: eng.tensor_tensor(
                out=G8, in0=pg[:, 0:8], in1=cst[:, O_BG:O_BG + 8], op=ALU.add),
                reads=(rpg, r_cst), writes=(r_sm,))
            P.add("act", lambda eng: eng.activation(out=E1, in_=sm[:, 4:8], func=AF.Exp, scale=-1.0),
                  reads=(r_sm,), writes=(r_sm,))
            P.add("act", lambda eng: eng.activation(out=SP4, in_=E1, func=AF.Ln, bias=1.0),
                  reads=(r_sm,), writes=(r_sm,))
            pb, rpb = mmbank()

            def f_b(eng, pb=pb):
                eng.matmul(pb[:, 0:4], lhsT=tri32, rhs=SP4, start=True, stop=True)
                return eng.matmul(pb[:, 4:8], lhsT=ones32[:, :], rhs=SP4, start=True, stop=True)

            P.add("pe", f_b, reads=(r_sm, r_cst, r_cbf), writes=(rpb,))
            P.add("dve", lambda eng, pb=pb: eng.tensor_tensor(
                out=T4, in0=pb[:, 0:4], in1=sm[:, 0:4], op=ALU.add),
                reads=(rpb, r_sm), writes=(r_sm,))
            P.add("dve", lambda eng, pb=pb: eng.tensor_tensor(
                out=T4B, in0=T4, in1=pb[:, 4:8], op=ALU.subtract),
                reads=(rpb, r_sm), writes=(r_sm,))
            P.add("act", lambda eng, tb=tb: eng.activation(
                out=gq[:, tb, 0:4], in_=T4, func=AF.Exp, bias=LN_KSCALE), reads=(r_sm,), writes=(r_gq,))
            P.add("act", lambda eng, tb=tb: eng.activation(
                out=gq[:, tb, 4:8], in_=T4B, func=AF.Exp, bias=LN_KSCALE), reads=(r_sm,), writes=(r_gq,))
            P.add("act", lambda eng, tb=tb, pb=pb: eng.activation(
                out=gq[:, tb, 8:12], in_=pb[:, 0:4], func=AF.Exp), reads=(rpb,), writes=(r_gq,))
            P.add("act", lambda eng, tb=tb, pb=pb: eng.activation(
                out=gq[:, tb, 12:16], in_=pb[:, 4:8], func=AF.Exp, scale=-1.0), reads=(rpb,), writes=(r_gq,))
        P.add("sp", lambda eng: eng.dma_start(out=sp_g[t], in_=gq[:, :, :].rearrange("p a b -> p (a b)")),
              reads=(r_gq,), writes=(res("dram_%d" % t),), dma=s_gq)

        wv, rw, _ = wload([wview(mwin_d, 512, 512)], KC)
        for tb in range(8):
            si = tb // 4
            tc0 = HALO + tb * 128
            pk, rpk = mmbank()

            def f_k(eng, pk=pk, tc0=tc0, wv=wv):
                out = None
                for k in range(KC):
                    out = eng.matmul(pk[:, 0:512], lhsT=xn[:, k, tc0:tc0 + 128], rhs=wv[:, k, 0:512],
                                     start=(k == 0), stop=(k == KC - 1))
                return out

            P.add("pe", f_k, reads=[rw] + [rX(k, si) for k in range(KC)], writes=(rpk,))
            for hh in range(NH):
                if tb % 2 == 0:
                    P.add("dve", lambda eng, pk=pk, tb=tb, hh=hh: eng.tensor_scalar(
                        out=kc2all(tb)[:, hh * 128:(hh + 1) * 128], in0=pk[:, hh * 128:(hh + 1) * 128],
                        scalar1=gq[:, tb, 4 + hh:5 + hh], scalar2=None, op0=ALU.mult),
                        reads=(rpk, r_gq), writes=(r_kc2[tb],))
                else:
                    P.add("act", lambda eng, pk=pk, tb=tb, hh=hh: eng.activation(
                        out=kc2all(tb)[:, hh * 128:(hh + 1) * 128], in_=pk[:, hh * 128:(hh + 1) * 128],
                        func=AF.Copy, scale=gq[:, tb, 4 + hh:5 + hh]),
                        reads=(rpk, r_gq), writes=(r_kc2[tb],))
            P.add("sp", lambda eng, tb=tb: eng.dma_start(out=sp_kc[t * 8 + tb], in_=kc2all(tb)),
                  reads=(r_kc2[tb],), writes=(res("dram_%d" % t),), dma=s_kc)

        vcnt = [0]
        pend = [None]
        for hp in range(2):
            wv, rw, _ = wload([wview(mwin_d, 1024 + hp * 512, 512)], KC)
            for tb in range(8):
                si = tb // 4
                tc0 = HALO + tb * 128
                pv, rpv = mmbank()
                b = vcnt[0] % 2
                vcnt[0] += 1

                def f_v(eng, pv=pv, tc0=tc0, wv=wv):
                    out = None
                    for k in range(KC):
                        out = eng.matmul(pv[:, 0:512], lhsT=xn[:, k, tc0:tc0 + 128], rhs=wv[:, k, 0:512],
                                         start=(k == 0), stop=(k == KC - 1))
                    return out

                P.add("pe", f_v, reads=[rw] + [rX(k, si) for k in range(KC)], writes=(rpv,))
                P.add("act", lambda eng, pv=pv, b=b: eng.activation(
                    out=vst[b][:, :, 0:DV], in_=pv[:, 0:512].rearrange("p (a b) -> p a b", a=2),
                    func=AF.Copy), reads=(rpv,), writes=(r_vs[b],))
                P.add("sp", lambda eng, b=b, tb=tb, hp=hp: eng.dma_start(
                    out=sp_v[t * 8 + tb][:, hp * 2 * DVE_:(hp + 1) * 2 * DVE_],
                    in_=vst[b][:, :, :].rearrange("p a b -> p (a b)")),
                    reads=(r_vs[b],), writes=(res("dram_%d" % t),), dma=s_vs[b])
                def scan(tb=tb, b=b, hp=hp):
                    for hl in range(2):
                        hh = hp * 2 + hl
                        pc, rpc = mmbank()
                        P.add("pe", lambda eng, pc=pc, tb=tb, hh=hh, hl=hl, b=b: eng.matmul(
                            pc[:, 0:DVE_], lhsT=kc2all(tb)[:, hh * 128:(hh + 1) * 128], rhs=vst[b][:, hl, :],
                            start=True, stop=True),
                            reads=(r_kc2[tb], r_vs[b]), writes=(rpc,))
                        P.add("dve", lambda eng, pc=pc, tb=tb, hh=hh: eng.scalar_tensor_tensor(
                            out=C32[:, hh, :], in0=C32[:, hh, :], scalar=gq[:, tb, 12 + hh:13 + hh],
                            in1=pc[:, 0:DVE_], op0=ALU.mult, op1=ALU.add),
                            reads=(rpc, r_gq, r_C[hh]), writes=(r_C[hh],))

                if pend[0] is not None:
                    pend[0]()
                pend[0] = scan
            pend[0]()
            pend[0] = None
        P.add("dve", lambda eng: eng.memset(sm[:, 60:61], 0.0),
              writes=r_kc2 + r_fm + r_vs + r_hid_all)

    def spill_h(t):
        s_h = P.dmasem("sph")
        P.add("sp", lambda eng: eng.dma_start(
            out=sp_h[t].rearrange("p (k n) -> p k n", k=KC), in_=h[:, :, HALO:HALO + TILE]),
            reads=[rH(k, s) for k in range(KC) for s in range(2)], writes=(res("dram_%d" % t),), dma=s_h)


    def mlstm_out(t):
        qT = xn[:, 0:4, HALO:HALO + TILE]
        kT = xn[:, 4:8, HALO:HALO + TILE]
        r_xn_all = [rX(k, s) for k in range(KC) for s in range(2)]
        r_qk = res("qk")
        s_qk = P.dmasem("qk")
        r_gq = res("gq")
        s_gq = P.dmasem("gq")
        r_kcc = [res("kc2c%d" % i) for i in range(3)]
        r_vc = [res("vc%d" % i) for i in range(3)]
        r_so = [res("soc%d" % i) for i in range(3)]
        s_ch = [P.dmasem("chunk%d" % i) for i in range(3)]
        r_C = [res("C32_%d" % hh) for hh in range(NH)]
        r_Cb = [res("Cbf_%d" % hh) for hh in range(NH)]
        r_PT = [res("PT0"), res("PT1")]
        r_hnb = [res("hnb%d" % i) for i in range(4)]
        r_pstc = r_pstb
        r_dram = res("dram_%d" % t)

        def f_qk(eng):
            return [eng.dma_start(out=qT, in_=sp_q[t].rearrange("p (a b) -> p a b", a=NH)),
                    eng.dma_start(out=kT, in_=sp_k[t].rearrange("p (a b) -> p a b", a=NH))]

        P.add("sp", f_qk, reads=(r_dram,), writes=[r_qk] + r_xn_all, dma=s_qk, ndma=2)
        P.add("sp", lambda eng: eng.dma_start(out=gq[:, :, :].rearrange("p a b -> p (a b)"), in_=sp_g[t]),
              reads=(r_dram,), writes=(r_gq,), dma=s_gq)

        def load_chunk(tb):
            b = tb % 3
            tc0 = tb * 128

            def f_ch(eng, b=b, tb=tb, tc0=tc0):
                return [eng.dma_start(out=kc2c[b][:, :], in_=sp_kc[t * 8 + tb]),
                        eng.dma_start(out=vc[b][:, :, :].rearrange("p a b -> p (a b)"), in_=sp_v[t * 8 + tb]),
                        eng.dma_start(out=soc[b][:, :, :],
                                      in_=sp_o[t].rearrange("p (a b) -> p a b", a=KC)[:, :, tc0:tc0 + 128])]

            P.add("sp", f_ch, reads=(r_dram,), writes=(r_kcc[b], r_vc[b], r_so[b]), dma=s_ch[b], ndma=3)

        units = [(tb, hh) for tb in range(8) for hh in range(NH)]
        NU = len(units)
        st = {}
        def stageA(i):
            tb, hh = units[i]
            tc0 = tb * 128
            pS, rpS = psb[i % 2], r_ps[i % 2]
            pb_ = i % 2
            P.add("pe", lambda eng: eng.matmul(
                pS[:, 0:128], lhsT=kT[:, hh, tc0:tc0 + 128], rhs=qT[:, hh, tc0:tc0 + 128],
                start=True, stop=True), reads=(r_qk,), writes=(rpS,))
            P.add("dve", lambda eng: eng.scalar_tensor_tensor(
                out=PT[pb_][:, :], in0=pS[:, 0:128], scalar=gq[:, tb, hh:hh + 1], in1=tri32,
                op0=ALU.mult, op1=ALU.mult),
                reads=(rpS, r_gq, r_cst), writes=(r_PT[pb_],))

        def stageB(i):
            tb, hh = units[i]
            tc0 = tb * 128
            b = tb % 3
            pb_ = i % 2
            pG, rpG = psb[3 + i % 3], r_ps[3 + i % 3]
            pC, rpC = psb[2], r_ps[2]
            hb = i % 4
            o = 24 + (i % 4) * 8
            DN, RD, SS, T1, LT, RS, FF = [sm[:, o + q:o + q + 1] for q in range(7)]
            r_s = res("sm_%d" % (i % 4))

            def f_G(eng):
                eng.matmul(pG[:, 0:DVE_], lhsT=PT[pb_][:, :], rhs=vc[b][:, hh, :], start=True, stop=False)
                return eng.matmul(pG[:, 0:DVE_], lhsT=qT[:, hh, tc0:tc0 + 128], rhs=Cbf[:, hh, :],
                                  start=False, stop=True)

            P.add("pe", f_G, reads=(r_PT[pb_], r_vc[b], r_qk, r_Cb[hh]), writes=(rpG,))
            P.add("pe", lambda eng: eng.matmul(
                pC[:, 0:DVE_], lhsT=kc2c[b][:, hh * 128:(hh + 1) * 128], rhs=vc[b][:, hh, :],
                start=True, stop=True), reads=(r_kcc[b], r_vc[b]), writes=(rpC,))
            P.add("dve", lambda eng: eng.tensor_tensor(
                out=T1, in0=pG[:, DV:DVE_], in1=gq[:, tb, 8 + hh:9 + hh], op=ALU.max),
                reads=(rpG, r_gq), writes=(r_s,))
            P.add("dve", lambda eng: eng.scalar_tensor_tensor(
                out=DN, in0=pG[:, DV:DVE_], scalar=-1.0, in1=T1, op0=ALU.mult, op1=ALU.max),
                reads=(rpG, r_s), writes=(r_s,))
            P.add("dve", lambda eng: eng.reciprocal(out=RD, in_=DN), reads=(r_s,), writes=(r_s,))
            P.add("act", lambda eng: eng.activation(
                out=hnb[hb][:, :], in_=pG[:, 0:DV], func=AF.Square, scale=RD, accum_out=SS),
                reads=(rpG, r_s), writes=(r_s, r_hnb[hb]))
            P.add("dve", lambda eng: eng.scalar_tensor_tensor(
                out=C32[:, hh, :], in0=C32[:, hh, :], scalar=gq[:, tb, 12 + hh:13 + hh],
                in1=pC[:, 0:DVE_], op0=ALU.mult, op1=ALU.add),
                reads=(rpC, r_gq, r_C[hh]), writes=(r_C[hh],))
            P.add("act", lambda eng: eng.activation(
                out=LT, in_=SS, func=AF.Ln, bias=float(EPS), scale=1.0 / DV), reads=(r_s,), writes=(r_s,))
            P.add("act", lambda eng: eng.activation(out=RS, in_=LT, func=AF.Exp, scale=-0.5),
                  reads=(r_s,), writes=(r_s,))
            P.add("dve", lambda eng: eng.tensor_tensor(out=FF, in0=RD, in1=RS, op=ALU.mult),
                  reads=(r_s,), writes=(r_s,))
            P.add("act", lambda eng: eng.activation(
                out=hnb[hb][:, :], in_=pG[:, 0:DV], func=AF.Copy, scale=FF),
                reads=(rpG, r_s), writes=(r_hnb[hb],))
            P.add("act", lambda eng: eng.activation(out=Cbf[:, hh, :], in_=C32[:, hh, :], func=AF.Copy),
                  reads=(r_C[hh],), writes=(r_Cb[hh],))

        def stageC(i):
            tb, hh = units[i]
            tc0 = tb * 128
            si = tb // 4
            b = tb % 3
            hb = i % 4
            pc_ = i % 2
            po = 0
            pst = pstb[pc_]

            def f_T(eng):
                eng.transpose(out=pst[:, po:po + 128], in_=hnb[hb][:, 0:128], identity=ident_bf[:, :])
                return eng.transpose(out=pst[:, po + 128:po + 256], in_=hnb[hb][:, 128:256],
                                     identity=ident_bf[:, :])

            P.add("pe", f_T, reads=(r_hnb[hb], r_cbf), writes=(r_pstc[pc_],))
            for j in range(2):
                c = hh * 2 + j
                P.add("dve", lambda eng, j=j, c=c: eng.scalar_tensor_tensor(
                    out=hid[:, c, HALO + tc0:HALO + tc0 + 128], in0=pst[:, po + j * 128:po + (j + 1) * 128],
                    scalar=cst[:, O_HN + c:O_HN + c + 1], in1=soc[b][:, c, :],
                    op0=ALU.mult, op1=ALU.mult),
                    reads=(r_pstc[pc_], r_cst, r_so[b]), writes=(rHid(c, si),))

        load_chunk(0)
        load_chunk(1)
        load_chunk(2)
        LAGB, LAGC = 1, 3
        for idx in range(NU + LAGC):
            if idx < NU:
                stageA(idx)
            if 0 <= idx - LAGB < NU:
                stageB(idx - LAGB)
            if 0 <= idx - LAGC < NU:
                stageC(idx - LAGC)
                tb_, hh_ = units[idx - LAGC]
                if hh_ == NH - 1 and tb_ + 3 < 8:
                    load_chunk(tb_ + 3)
        P.add("dve", lambda eng: eng.memset(sm[:, 61:62], 0.0), writes=[r_qk] + r_xn_all)

    r_ost = r_xst
    s_ost = s_xst

    def final_out(t, pre=False):
        subs = [(0, SUBS[0]), (1, SUBS[1])]
        if not pre:
            rmsnorm(O_GF, subs, out_bf=False)
        for tb in range(8):
            if tb == 2:
                flush_carry()
            b = xcnt[0] % 4
            xcnt[0] += 1
            si = tb // 4
            c0 = HALO + tb * 128
            for g in range(2):
                pT, rpT = mmbank()

                def f_tr(eng, g=g, c0=c0, pT=pT):
                    out = None
                    for q in range(4):
                        kc = g * 4 + q
                        out = eng.transpose(out=pT[:, q * 128:(q + 1) * 128],
                                            in_=h[:, kc, c0:c0 + 128], identity=ident32)
                    return out

                P.add("pe", f_tr, reads=[rH(g * 4 + q, si) for q in range(4)] + [r_cst], writes=(rpT,))
                if g == 0:
                    P.add("act", lambda eng, b=b, pT=pT: eng.activation(out=xst[b][:, 0:512], in_=pT[:, :], func=AF.Copy),
                          reads=(rpT,), writes=(r_ost[b],))
                else:
                    P.add("dve", lambda eng, b=b, pT=pT: eng.tensor_copy(out=xst[b][:, 512:1024], in_=pT[:, :]),
                          reads=(rpT,), writes=(r_ost[b],))
            row0 = t * TILE + tb * 128
            P.add("sp", lambda eng, b=b, row0=row0: eng.dma_start(out=out_d[row0:row0 + 128, :], in_=xst[b][:, :]),
                  reads=(r_ost[b],), dma=s_ost[b])

    r_Call = [res("C32_%d" % hh) for hh in range(NH)]
    main = [(0, SUBS[0]), (1, SUBS[1])]
    with_halo = [(2, HALO_SUB)] + main
    if do1:
        P.add("dve", lambda eng: eng.memset(C32[:, :, :], 0.0), writes=r_Call)
        for t in range(NT):
            if t == 0:
                load_x(t)
            subs0 = with_halo if t == 0 else main
            G = lambda l_, i_: (O_G + (l_ * 3 + i_) * 8, True)
            if t == 0:
                ffn(0, 0, subs0)
                conv_mixer(t, subs0, nxt=G(0, 2))
            else:
                ffn(0, 0, main, nxt=G(0, 1))
                conv_mixer(t, main, pre=True, nxt=G(0, 2))
            ffn(0, 1, main, pre=True, nxt=G(1, 0))
            ffn(1, 0, main, pre=True, nxt=G(1, 1))
            mlstm_inproj(t, pre=True,
                         hook=(lambda m, t=t: load_x_tb(t + 1, m)) if t + 1 < NT else None)
    s_st = P.dmasem("state")
    if mode == "p1":
        P.add("sp", lambda eng: eng.dma_start(out=st_out[:, :], in_=C32[:, :, :].rearrange("p a b -> p (a b)")),
              reads=r_Call, dma=s_st)
    if mode == "p2":
        P.add("sp", lambda eng: eng.dma_start(out=C32[:, :, :].rearrange("p a b -> p (a b)"), in_=st_in[:, :]),
              writes=r_Call, dma=s_st)
    if mode == "fused":
        r_stl, r_sta = res("st_loc"), res("st_all")
        s_cc = P.dmasem("cc", inc=1)
        s_st2 = P.dmasem("state2")
        P.add("sp", lambda eng: eng.dma_start(out=st_loc[:, :], in_=C32[:, :, :].rearrange("p a b -> p (a b)")),
              reads=r_Call, writes=(r_stl,), dma=s_st)
        P.add("pool", lambda eng: eng.collective_compute(
            "AllGather", ALU.bypass, replica_groups=[[0, 1], [2, 3], [4, 5], [6, 7]],
            ins=[st_loc[:, :]], outs=[st_all[:, :]]),
            reads=(r_stl,), writes=(r_sta,), dma=s_cc)
        P.add("sp", lambda eng: eng.dma_start(out=C32[:, :, :].rearrange("p a b -> p (a b)"), in_=st_all[0:128, :]),
              reads=(r_sta,), writes=r_Call, dma=s_st2)
        P.add("dve", lambda eng: eng.tensor_scalar(
            out=C32[:, :, :], in0=C32[:, :, :], scalar1=cst[:, O_MASK:O_MASK + 1], scalar2=None, op0=ALU.mult),
            reads=r_Call + [r_cst], writes=r_Call)
    if do2:
        r_Cb = [res("Cbf_%d" % hh) for hh in range(NH)]
        for hh in range(NH):
            P.add("act", lambda eng, hh=hh: eng.activation(out=Cbf[:, hh, :], in_=C32[:, hh, :], func=AF.Copy),
                  reads=(r_Call[hh],), writes=(r_Cb[hh],))
        for b in range(3):
            P.add("dve", lambda eng, b=b: eng.memset(vc[b][:, :, :], 1.0), writes=(res("vc%d" % b),))
        s_h = P.dmasem("sph")
        for t in range(NT):
            P.add("sp", lambda eng, t=t: eng.dma_start(
                out=h[:, :, HALO:HALO + TILE], in_=sp_h[t].rearrange("p (k n) -> p k n", k=KC)),
                reads=(res("dram_%d" % t),),
                writes=[rH(k, s) for k in range(KC) for s in range(2)], dma=s_h)
            mlstm_out(t)
            out_proj(mwout_d, main, nxt=(O_G + 5 * 8, True))
            ffn(1, 1, main, pre=True, nxt=(O_GF, False))
            final_out(t, pre=True)
            flush_carry()
    allres = list(R.values())
    P.add("sp", lambda eng: eng.nop(), reads=allres, writes=allres)
    P.emit(nc, es)
    es.close()
    return nc


def _consts(norm_g, final_norm_g, conv_w, head_norm, b_gates):
    c = np.zeros((128, NCONST), np.float32)
    c[:, O_ID:O_ID + 128] = np.eye(128, dtype=np.float32)
    c[:, O_TRI:O_TRI + 128] = np.triu(np.ones((128, 128), np.float32))
    c[:, O_G:O_G + 48] = norm_g.reshape(6, KC, 128).transpose(2, 0, 1).reshape(128, 48)
    c[:, O_GF:O_GF + 8] = final_norm_g.reshape(KC, 128).T
    c[:, O_CW:O_CW + 24] = conv_w.reshape(3, KC, 128).transpose(2, 0, 1).reshape(128, 24)
    c[:, O_HN:O_HN + 8] = head_norm.reshape(KC, 128).T
    c[:, O_BG:O_BG + 8] = np.broadcast_to(b_gates.reshape(1, 8), (128, 8))
    return c


_NC_CACHE = {}


def _get_nc(mode):
    if mode not in _NC_CACHE:
        _NC_CACHE[mode] = build(mode)
    return _NC_CACHE[mode]


MODE = "fused"


def kernel(x, norm_g, ffn_w_gate, ffn_w_up, ffn_w_down, conv_w_in, conv_w, conv_w_out,
           mlstm_w_in, mlstm_b_gates, mlstm_head_norm, mlstm_w_out, final_norm_g):
    f32 = lambda a: np.ascontiguousarray(np.asarray(a, dtype=np.float32))
    x = f32(x)
    consts = _consts(f32(norm_g), f32(final_norm_g), f32(conv_w[0]), f32(mlstm_head_norm[0]),
                     f32(mlstm_b_gates[0]))
    wg, wu, wd = f32(ffn_w_gate), f32(ffn_w_up), f32(ffn_w_down)
    xs = []
    for c in range(NCORES):
        b, half = c // 2, c % 2
        xc = np.zeros((TOK + HALO, D), np.float32)
        xc[HALO:] = x[b, half * TOK:(half + 1) * TOK]
        if half == 1:
            xc[:HALO] = x[b, TOK - HALO:TOK]
        xs.append(xc)
    common = {"ffn_w_gate": wg, "ffn_w_up": wu, "ffn_w_down": wd}
    cs = []
    for c in range(NCORES):
        cc_ = consts.copy()
        cc_[:, O_MASK] = float(c % 2)
        cs.append(cc_)
    p1_w = {"conv_w_in": f32(conv_w_in[0]), "conv_w_out": f32(conv_w_out[0]), "mlstm_w_in": f32(mlstm_w_in[0])}
    p2_w = {"mlstm_w_out": f32(mlstm_w_out[0])}
    cores = list(range(NCORES))
    if MODE == "two":
        nc1 = _get_nc("p1")
        r1 = run_bass_kernel_spmd(nc1, [dict(common, **p1_w, x=xs[c], consts=cs[c]) for c in cores], core_ids=cores).results
        nc2 = _get_nc("p2")
        in2 = []
        for c in cores:
            d = dict(common, **p2_w, consts=cs[c])
            for k in ("sp_h", "sp_q", "sp_k", "sp_o", "sp_kc", "sp_v", "sp_g"):
                d[k] = r1[c][k]
            d["st_in"] = (np.zeros((128, NH * DVE_), np.float32) if c % 2 == 0
                          else np.ascontiguousarray(r1[c - 1]["st_out"]))
            in2.append(d)
        r2 = run_bass_kernel_spmd(nc2, in2, core_ids=cores).results
        outs = [r2[c]["out"] for c in cores]
    else:
        ncf = _get_nc("fused")
        r = run_bass_kernel_spmd(ncf, [dict(common, **p1_w, **p2_w, x=xs[c], consts=cs[c]) for c in cores],
                                 core_ids=cores).results
        outs = [r[c]["out"] for c in cores]
    out = np.zeros((4, SEQ, D), np.float32)
    for c in cores:
        b, half = c // 2, c % 2
        out[b, half * TOK:(half + 1) * TOK] = np.asarray(outs[c], dtype=np.float32).reshape(TOK, D)
    return out
```

```python
import numpy as np
import ml_dtypes
from contextlib import ExitStack
import concourse.bass as bass
import concourse.mybir as mybir
from concourse.bass_utils import run_bass_kernel_spmd

F32 = mybir.dt.float32
BF16 = mybir.dt.bfloat16
ALU = mybir.AluOpType
AF = mybir.ActivationFunctionType

NCORES = 8
D = 1024
KC = 8
DFF = 2816
SEQ = 8192
TOK = 4096
TILE = 1024
HALO = 2
TC = TILE + HALO
NT = TOK // TILE
SUB = 512
NH = 4
DQK = 128
DV = 256
DVE_ = DV + 1
MIN_DIM = 3080
EPS = 1e-6
LN_KSCALE = float(np.log(DQK ** -0.5))

O_ID = 0
O_TRI = 128
O_G = 256
O_GF = 304
O_CW = 312
O_HN = 336
O_BG = 344
O_MASK = 352
O_BG8 = 356
NCONST = 420

WSLOT = 5632
NWS = 4


class Res:
    __slots__ = ("name", "w", "r", "excl")

    def __init__(self, name, excl=False):
        self.name = name
        self.w = None
        self.r = []
        self.excl = excl


class DmaSem:
    __slots__ = ("name", "count", "sem", "inc")

    def __init__(self, name, inc=16):
        self.name = name
        self.count = 0
        self.sem = None
        self.inc = inc


class Op:
    __slots__ = ("eng", "fn", "deps", "idx", "dma", "ndma", "needs_inc", "val")


class Prog:
    ENGS = ("pe", "act", "dve", "pool", "sp")

    def __init__(self):
        self.ops = []
        self.dmasems = []

    def dmasem(self, name, inc=16):
        for s in self.dmasems:
            if s.name == name:
                return s
        s = DmaSem(name, inc)
        self.dmasems.append(s)
        return s

    def add(self, eng, fn, reads=(), writes=(), dma=None, ndma=1):
        op = Op()
        op.eng = eng
        op.fn = fn
        op.idx = len(self.ops)
        op.dma = dma
        op.ndma = ndma
        op.needs_inc = False
        op.val = 0
        deps = set()
        for r in reads:
            if r.w is not None:
                deps.add(r.w)
            if r.excl:
                deps.update(x for x in r.r if self.ops[x].eng != eng)
        for r in writes:
            if r.w is not None:
                deps.add(r.w)
            deps.update(r.r)
        for r in reads:
            r.r.append(op.idx)
        for r in writes:
            r.w = op.idx
            r.r = []
        deps.discard(op.idx)
        if eng == "pe" and dma is None:
            deps = {d for d in deps if not (self.ops[d].eng == "pe" and self.ops[d].dma is None)}
        op.deps = deps
        self.ops.append(op)
        return op

    def emit(self, nc, es):
        ops = self.ops
        for op in ops:
            for d in op.deps:
                ops[d].needs_inc = True
        cnt = {e: 0 for e in self.ENGS}
        for op in ops:
            if op.dma is not None:
                op.dma.count += op.dma.inc * op.ndma
                op.val = op.dma.count
            elif op.needs_inc:
                cnt[op.eng] += 1
                op.val = cnt[op.eng]
        esem = {e: es.enter_context(nc.semaphore("s_" + e)) for e in self.ENGS}
        for s in self.dmasems:
            if s.count > 0:
                s.sem = es.enter_context(nc.semaphore("d_" + s.name))
        block = es.enter_context(nc.Block())
        starters = {"pe": block.tensor, "act": block.scalar, "dve": block.vector,
                    "pool": block.gpsimd, "sp": block.sync}
        for e in self.ENGS:
            eops = [op for op in ops if op.eng == e]
            if not eops:
                continue

            def body(eng, eops=eops, e=e):
                waited = {}
                for op in eops:
                    need = {}
                    for d in op.deps:
                        dop = ops[d]
                        if dop.dma is not None:
                            key = ("d", dop.dma.name)
                            sem = dop.dma.sem
                        else:
                            key = ("e", dop.eng)
                            sem = esem[dop.eng]
                        if need.get(key, (None, 0))[1] < dop.val:
                            need[key] = (sem, dop.val)
                    for key, (sem, val) in need.items():
                        if waited.get(key, 0) < val:
                            eng.wait_ge(sem, val)
                            waited[key] = val
                    res = op.fn(eng)
                    if op.dma is not None:
                        insts = res if isinstance(res, (list, tuple)) else [res]
                        assert len(insts) == op.ndma, (len(insts), op.ndma)
                        for i_ in insts:
                            if op.dma.inc == 16:
                                i_.then_inc(op.dma.sem, 16)
                            else:
                                i_.then_inc(op.dma.sem)
                    elif op.needs_inc:
                        inst = res[-1] if isinstance(res, (list, tuple)) else res
                        inst.then_inc(esem[e], 1)

            starters[e](body)


def build(mode):
    do1 = mode in ("p1", "fused")
    do2 = mode in ("p2", "fused")
    nc = bass.Bass("TRN2", target_bir_lowering=False)
    P = Prog()
    es = ExitStack()

    def dram(name, shape, dt, kind):
        return nc.dram_tensor(name, list(shape), dt, kind=kind).ap()

    consts_d = dram("consts", [128, NCONST], F32, "ExternalInput")
    wgate_d = dram("ffn_w_gate", [2, 2, D, DFF], F32, "ExternalInput")
    wup_d = dram("ffn_w_up", [2, 2, D, DFF], F32, "ExternalInput")
    wdown_d = dram("ffn_w_down", [2, 2, DFF, D], F32, "ExternalInput")
    if do1:
        x_d = dram("x", [TOK + HALO, D], F32, "ExternalInput")
        cwin_d = dram("conv_w_in", [D, 3 * D], F32, "ExternalInput")
        cwout_d = dram("conv_w_out", [D, D], F32, "ExternalInput")
        mwin_d = dram("mlstm_w_in", [D, MIN_DIM], F32, "ExternalInput")
    if do2:
        mwout_d = dram("mlstm_w_out", [D, D], F32, "ExternalInput")
        out_d = dram("out", [TOK, D], F32, "ExternalOutput")
    k1 = "ExternalOutput" if mode == "p1" else ("ExternalInput" if mode == "p2" else "Internal")
    sp_h = dram("sp_h", [NT, 128, KC * TILE], F32, k1)
    sp_q = dram("sp_q", [NT, 128, NH * TILE], BF16, k1)
    sp_k = dram("sp_k", [NT, 128, NH * TILE], BF16, k1)
    sp_o = dram("sp_o", [NT, 128, KC * TILE], BF16, k1)
    sp_kc = dram("sp_kc", [NT * 8, 128, NH * DQK], BF16, k1)
    sp_v = dram("sp_v", [NT * 8, 128, NH * DVE_], BF16, k1)
    sp_g = dram("sp_g", [NT, 128, 8 * 16], F32, k1)
    if mode == "p1":
        st_out = dram("st_out", [128, NH * DVE_], F32, "ExternalOutput")
    if mode == "p2":
        st_in = dram("st_in", [128, NH * DVE_], F32, "ExternalInput")
    if mode == "fused":
        st_loc = dram("st_loc", [128, NH * DVE_], F32, "Internal")
        st_all = dram("st_all", [256, NH * DVE_], F32, "Internal")

    def sb(name, shape, dt):
        return es.enter_context(nc.sbuf_tensor(name, list(shape), dt))

    cst = sb("cst", [128, NCONST], F32)
    ident_bf = sb("ident_bf", [128, 128], BF16)
    ones_bf = sb("ones_bf", [128, 128], BF16)
    ones32 = sb("ones32", [128, 128], F32)
    h = sb("h", [128, KC, TC], F32)
    xn = sb("xn", [128, KC, TC], BF16)
    hid = sb("hid", [128, 11, TC], BF16)
    wr = [sb("wr%d" % i, [128, WSLOT], BF16) for i in range(NWS)]
    sq = sb("sq", [128, KC, SUB], BF16)
    rstd = sb("rstd", [128, TC], F32)
    lnt = sb("lnt", [128, SUB], F32)
    gact = [sb("gact%d" % i, [128, SUB], F32) for i in range(2)]
    xst = [sb("xst%d" % i, [128, D], F32) for i in range(4)]
    gq = sb("gq", [128, 8, 16], F32)
    C32 = sb("C32", [128, NH, DVE_], F32)
    sm = sb("sm", [128, 64], F32)
    gsc = sb("gsc", [128, 160], F32)
    if do1:
        gbs = sb("gbs", [128, TC], F32)
        acc = sb("acc", [128, TILE], F32)
        ucarry = sb("ucarry", [128, KC, 2], F32)
    if do2:
        kc2c = [sb("kc2c%d" % i, [128, NH * DQK], BF16) for i in range(3)]
        vc = [sb("vc%d" % i, [128, NH, DVE_], BF16) for i in range(3)]
        soc = [sb("soc%d" % i, [128, KC, 128], BF16) for i in range(3)]
        Cbf = sb("Cbf", [128, NH, DVE_], BF16)
        PT = [sb("PT%d" % i, [128, 128], BF16) for i in range(2)]
        hnb = [sb("hnb%d" % i, [128, DV], BF16) for i in range(4)]

    psb = [es.enter_context(nc.psum_tensor("ps%d" % i, [128, 512], F32)) for i in range(6)]
    pstb = [es.enter_context(nc.psum_tensor("pst%d" % i, [128, 1024], BF16)) for i in range(2)]

    R = {}

    def res(name):
        if name not in R:
            R[name] = Res(name)
        return R[name]

    for i_ in range(6):
        R["ps%d" % i_] = Res("ps%d" % i_, excl=True)
    for i_ in range(2):
        R["pst%d" % i_] = Res("pst%d" % i_, excl=True)
    r_ps = [res("ps%d" % i) for i in range(6)]
    r_pstb = [res("pst0"), res("pst1")]
    r_cst = res("cst")
    r_wr = [res("wr%d" % i) for i in range(NWS)]
    s_wr = [P.dmasem("wr%d" % i) for i in range(NWS)]

    SUBS = [(HALO, SUB), (HALO + SUB, SUB)]
    HALO_SUB = (0, HALO)

    def rH(kc, s):
        return res("h_%d_%d" % (kc, s))

    def rX(kc, s):
        return res("xn_%d_%d" % (kc, s))

    def rHid(j, s):
        return res("hid_%d_%d" % (j, s))

    mmrot = [0]

    def mmbank():
        i = mmrot[0] % 6
        mmrot[0] += 1
        return psb[i], r_ps[i]


    wcount = [0]

    def wload(pieces, kc):
        i = wcount[0] % NWS
        wcount[0] += 1
        offs = []
        o = 0
        for ap_ in pieces:
            offs.append(o)
            o += ap_.shape[2]
        tot = o
        assert kc * tot <= WSLOT

        def fn(eng, i=i, pieces=pieces, offs=offs, tot=tot, kc=kc):
            insts = []
            dst = wr[i][:, 0:kc * tot].rearrange("p (k n) -> p k n", k=kc)
            for ap_, o_ in zip(pieces, offs):
                insts.append(eng.dma_start(out=dst[:, :, o_:o_ + ap_.shape[2]], in_=ap_))
            return insts

        P.add("pool", fn, reads=(), writes=(r_wr[i],), dma=s_wr[i], ndma=len(pieces))
        view = wr[i][:, 0:kc * tot].rearrange("p (k n) -> p k n", k=kc)
        return view, r_wr[i], offs

    def wview(wd, c0, cw):
        return wd.rearrange("(k p) n -> p k n", p=128)[:, :, c0:c0 + cw]

    s_cst = P.dmasem("cst")
    P.add("sp", lambda eng: eng.dma_start(out=cst[:, :], in_=consts_d[:, :]),
          writes=(r_cst,), dma=s_cst)
    r_cbf = res("cbf")
    P.add("dve", lambda eng: eng.tensor_copy(out=ident_bf[:, :], in_=cst[:, O_ID:O_ID + 128]),
          reads=(r_cst,), writes=(r_cbf,))
    P.add("dve", lambda eng: eng.memset(ones_bf[:, :], 1.0), writes=(r_cbf,))
    P.add("dve", lambda eng: eng.memset(ones32[:, :], 1.0), writes=(r_cbf,))
    ident32 = cst[:, O_ID:O_ID + 128]
    tri32 = cst[:, O_TRI:O_TRI + 128]

    carry = []

    def flush_carry():
        while carry:
            carry.pop(0)()

    def norm_parts(gcol, si, c0, n, out_bf=True):
        r_sq = res("sq")
        r_lnt = res("lnt")
        r_rs = res("rstd_%d" % si)

        def partA():
            P.add("act", lambda eng: eng.activation(out=sq[:, :, 0:n], in_=h[:, :, c0:c0 + n], func=AF.Square),
                  reads=[rH(k, si) for k in range(KC)], writes=(r_sq,))

        def partB():
            AUX, r_AUX = mmbank()

            def f_mm(eng):
                out = None
                for k in range(KC):
                    out = eng.matmul(AUX[:, 0:n], lhsT=ones_bf[:, :], rhs=sq[:, k, 0:n],
                                     start=(k == 0), stop=(k == KC - 1))
                return out

            P.add("pe", f_mm, reads=(r_sq, r_cbf), writes=(r_AUX,))
            P.add("act", lambda eng: eng.activation(out=lnt[:, 0:n], in_=AUX[:, 0:n], func=AF.Ln,
                                                    bias=float(EPS), scale=1.0 / D),
                  reads=(r_AUX,), writes=(r_lnt,))
            P.add("act", lambda eng: eng.activation(out=rstd[:, c0:c0 + n], in_=lnt[:, 0:n], func=AF.Exp,
                                                    scale=-0.5),
                  reads=(r_lnt,), writes=(r_rs,))
            for kc in range(KC):
                dst = xn if out_bf else h
                r_dst = rX(kc, si) if out_bf else rH(kc, si)
                P.add("dve", lambda eng, kc=kc, dst=dst: eng.scalar_tensor_tensor(
                    out=dst[:, kc, c0:c0 + n], in0=h[:, kc, c0:c0 + n],
                    scalar=cst[:, gcol + kc:gcol + kc + 1], in1=rstd[:, c0:c0 + n],
                    op0=ALU.mult, op1=ALU.mult),
                    reads=(rH(kc, si), r_rs, r_cst), writes=(r_dst,))

        return partA, partB

    def rmsnorm(gcol, subs, out_bf=True):
        for si, (c0, n) in subs:
            a, b = norm_parts(gcol, si, c0, n, out_bf)
            a()
            b()

    def final_update(blocks, kcn, rhs_fn, rhs_res, evac, subs, nxt):
        groups = [(mb, mm) for mb in range(len(blocks)) for mm in range(4)]

        def one(si, s0, n, mb, mm):
            wv, rw = blocks[mb]
            m = mb * 4 + mm
            pd, rpd = mmbank()
            mm_group(pd, rpd, wv, rw, mm * 128, kcn, lambda k: rhs_fn(k, s0, n), rhs_res(si), n)
            evac(pd, rpd, m, si, s0, n)

        if nxt is None or len(subs) != 2:
            for (mb, mm) in groups:
                for si, (s0, n) in subs:
                    one(si, s0, n, mb, mm)
            return
        (sa, (a0, an)), (sb_, (b0, bn)) = subs
        A0, B0 = norm_parts(nxt[0], sa, a0, an, nxt[1])
        A1, B1 = norm_parts(nxt[0], sb_, b0, bn, nxt[1])
        for (mb, mm) in groups:
            one(sa, a0, an, mb, mm)
        A0()
        for gi, (mb, mm) in enumerate(groups):
            one(sb_, b0, bn, mb, mm)
            if gi == 2:
                B0()
        A1()
        carry.append(B1)

    def mm_group(ps, r_p, wv, r_w, wcol, kcn, rhs_fn, rhs_res, n, mcols=128):
        def fn(eng):
            out = None
            for k in range(kcn):
                out = eng.matmul(ps[0:mcols, 0:n], lhsT=wv[:, k, wcol:wcol + mcols], rhs=rhs_fn(k),
                                 start=(k == 0), stop=(k == kcn - 1))
            return out

        P.add("pe", fn, reads=[r_w] + list(rhs_res), writes=(r_p,))

    def ffn(l, i, subs, pre=False, nxt=None):
        gcol = O_G + (l * 3 + (0 if i == 0 else 2)) * 8
        if not pre:
            rmsnorm(gcol, subs)
        gbuf = [0]
        r_ga = [res("gact0"), res("gact1")]
        for hh in range(2):
            j0 = hh * 11
            for (jb, nj) in ((0, 4), (4, 4), (8, 3)):
                c0 = (j0 + jb) * 128
                wg, rg, _ = wload([wview(wgate_d[l, i], c0, nj * 128)], KC)
                wu, ru, _ = wload([wview(wup_d[l, i], c0, nj * 128)], KC)
                for si, (s0, n) in subs:
                    for jj in range(nj):
                        j = jb + jj
                        pg, rpg = mmbank()
                        pu, rpu = mmbank()
                        xr = [rX(k, si) for k in range(KC)]
                        mm_group(pg, rpg, wg, rg, jj * 128, KC,
                                 lambda k, s0=s0, n=n: xn[:, k, s0:s0 + n], xr, n)
                        mm_group(pu, rpu, wu, ru, jj * 128, KC,
                                 lambda k, s0=s0, n=n: xn[:, k, s0:s0 + n], xr, n)
                        b = gbuf[0] % 2
                        gbuf[0] += 1
                        P.add("act", lambda eng, pg=pg, n=n, b=b: eng.activation(
                            out=gact[b][:, 0:n], in_=pg[:, 0:n], func=AF.Silu),
                            reads=(rpg,), writes=(r_ga[b],))
                        P.add("dve", lambda eng, pu=pu, n=n, b=b, j=j, s0=s0: eng.tensor_tensor(
                            out=hid[:, j, s0:s0 + n], in0=pu[:, 0:n], in1=gact[b][:, 0:n],
                            op=ALU.mult),
                            reads=(rpu, r_ga[b]), writes=(rHid(j, si),))
                        if jj == 1:
                            flush_carry()
            def evac_down(pd, rpd, m, si, s0, n):
                P.add("dve", lambda eng: eng.scalar_tensor_tensor(
                    out=h[:, m, s0:s0 + n], in0=pd[:, 0:n], scalar=0.5,
                    in1=h[:, m, s0:s0 + n], op0=ALU.mult, op1=ALU.add),
                    reads=(rpd, rH(m, si)), writes=(rH(m, si),))

            blocks = []
            for mb in range(2):
                wdv, rd, _ = wload([wdown_d[l, i][j0 * 128:(j0 + 11) * 128, :].rearrange(
                    "(k p) n -> p k n", p=128)[:, :, mb * 512:(mb + 1) * 512]], 11)
                blocks.append((wdv, rd))
            final_update(blocks, 11, lambda k, s0, n: hid[:, k, s0:s0 + n],
                         lambda si: [rHid(k, si) for k in range(11)], evac_down, subs,
                         nxt if hh == 1 else None)

    def out_proj(wd, subs, nxt=None):
        def evac_add(pd, rpd, m, si, s0, n):
            P.add("dve", lambda eng: eng.tensor_tensor(
                out=h[:, m, s0:s0 + n], in0=pd[:, 0:n], in1=h[:, m, s0:s0 + n], op=ALU.add),
                reads=(rpd, rH(m, si)), writes=(rH(m, si),))

        blocks = []
        for mb in range(2):
            wv, rw, _ = wload([wview(wd, mb * 512, 512)], KC)
            blocks.append((wv, rw))
        final_update(blocks, KC, lambda k, s0, n: hid[:, k, s0:s0 + n],
                     lambda si: [rHid(k, si) for k in range(KC)], evac_add, subs, nxt)

    r_xst = [res("xst%d" % i) for i in range(4)]
    s_xst = [P.dmasem("xst%d" % i) for i in range(4)]
    xcnt = [0]

    def load_x(t):
        for tb in range(8):
            load_x_tb(t, tb)
        if t == 0:
            load_x_halo()

    def load_x_tb(t, tb):
        if True:
            b = xcnt[0] % 4
            xcnt[0] += 1
            row0 = HALO + t * TILE + tb * 128
            P.add("sp", lambda eng, b=b, row0=row0: eng.dma_start(
                out=xst[b][:, :], in_=x_d[row0:row0 + 128, :]), writes=(r_xst[b],), dma=s_xst[b])
            si = tb // 4
            c0 = HALO + tb * 128
            for g in range(2):
                pT, rpT = mmbank()

                def f_tr(eng, b=b, g=g, pT=pT):
                    out = None
                    for q in range(4):
                        kc = g * 4 + q
                        out = eng.transpose(out=pT[:, q * 128:(q + 1) * 128],
                                            in_=xst[b][:, kc * 128:(kc + 1) * 128],
                                            identity=ident32)
                    return out

                P.add("pe", f_tr, reads=(r_xst[b], r_cst), writes=(rpT,))
                P.add("act" if g == 0 else "dve", lambda eng, g=g, c0=c0, pT=pT: (
                    eng.activation(out=h[:, g * 4:g * 4 + 4, c0:c0 + 128],
                                   in_=pT[:, :].rearrange("p (a b) -> p a b", a=4), func=AF.Copy)
                    if g == 0 else
                    eng.tensor_copy(out=h[:, g * 4:g * 4 + 4, c0:c0 + 128],
                                    in_=pT[:, :].rearrange("p (a b) -> p a b", a=4))),
                    reads=(rpT,), writes=[rH(g * 4 + q, si) for q in range(4)])
    def load_x_halo():
        if True:
            b = xcnt[0] % 4
            xcnt[0] += 1
            P.add("sp", lambda eng, b=b: eng.dma_start(
                out=xst[b][0:HALO, :], in_=x_d[0:HALO, :]), writes=(r_xst[b],), dma=s_xst[b])

            AUX, r_AUX = mmbank()

            def f_trh(eng, b=b):
                out = None
                for kc in range(KC):
                    out = eng.transpose(out=AUX[:, kc * HALO:(kc + 1) * HALO],
                                        in_=xst[b][0:HALO, kc * 128:(kc + 1) * 128],
                                        identity=cst[0:HALO, O_ID:O_ID + HALO])
                return out

            P.add("pe", f_trh, reads=(r_xst[b], r_cst), writes=(r_AUX,))
            P.add("dve", lambda eng: eng.tensor_copy(
                out=h[:, :, 0:HALO],
                in_=AUX[:, 0:KC * HALO].rearrange("p (a b) -> p a b", a=KC)),
                reads=(r_AUX,), writes=[rH(k, 2) for k in range(KC)])

    def conv_mixer(t, subs, pre=False, nxt=None):
        if not pre:
            rmsnorm(O_G + 1 * 8, subs)
        r_gc = [res("gact0"), res("gact1")]
        r_u = res("u")
        r_gb = res("gbs")
        r_acc = res("acc")
        r_uc = res("ucarry")
        u = rstd
        r_rs_all = [res("rstd_%d" % si) for si in range(3)]
        gbuf = [0]
        for cg in range(2):
            wvb, rwb, _ = wload([wview(cwin_d, 0 * D + cg * 512, 512)], KC)
            wvc, rwc, _ = wload([wview(cwin_d, 1 * D + cg * 512, 512)], KC)
            wvh, rwh, _ = wload([wview(cwin_d, 2 * D + cg * 512, 512)], KC)
            for cc in range(4):
                c = cg * 4 + cc
                if t > 0:
                    P.add("dve", lambda eng, c=c: eng.tensor_copy(out=u[:, 0:HALO], in_=ucarry[:, c, :]),
                          reads=(r_uc,), writes=[r_u] + r_rs_all)
                for si, (s0, n) in subs:
                    xr = [rX(k, si) for k in range(KC)]
                    rhs = lambda k, s0=s0, n=n: xn[:, k, s0:s0 + n]
                    pc, rpc = mmbank()
                    ph, rph = mmbank()
                    pb, rpb = mmbank()
                    mm_group(pc, rpc, wvc, rwc, cc * 128, KC, rhs, xr, n)
                    mm_group(ph, rph, wvh, rwh, cc * 128, KC, rhs, xr, n)
                    mm_group(pb, rpb, wvb, rwb, cc * 128, KC, rhs, xr, n)
                    b = gbuf[0] % 2
                    gbuf[0] += 1
                    P.add("act", lambda eng, pc=pc, n=n, b=b: eng.activation(
                        out=gact[b][:, 0:n], in_=pc[:, 0:n], func=AF.Copy),
                        reads=(rpc,), writes=(r_gc[b],))
                    P.add("dve", lambda eng, ph=ph, n=n, b=b, s0=s0: eng.tensor_tensor(
                        out=u[:, s0:s0 + n], in0=ph[:, 0:n], in1=gact[b][:, 0:n], op=ALU.mult),
                        reads=(rph, r_gc[b]), writes=[r_u] + r_rs_all)
                    P.add("act", lambda eng, pb=pb, n=n, s0=s0: eng.activation(
                        out=gbs[:, s0:s0 + n], in_=pb[:, 0:n], func=AF.Copy),
                        reads=(rpb,), writes=(r_gb,))
                    flush_carry()
                P.add("dve", lambda eng, c=c: eng.tensor_scalar(
                    out=acc[:, :], in0=u[:, 0:TILE], scalar1=cst[:, O_CW + c:O_CW + c + 1],
                    scalar2=None, op0=ALU.mult),
                    reads=(r_u, r_cst), writes=(r_acc,))
                P.add("dve", lambda eng, c=c: eng.scalar_tensor_tensor(
                    out=acc[:, :], in0=u[:, 1:TILE + 1], scalar=cst[:, O_CW + 8 + c:O_CW + 8 + c + 1],
                    in1=acc[:, :], op0=ALU.mult, op1=ALU.add),
                    reads=(r_u, r_cst, r_acc), writes=(r_acc,))
                P.add("dve", lambda eng, c=c: eng.scalar_tensor_tensor(
                    out=acc[:, :], in0=u[:, 2:TILE + 2], scalar=cst[:, O_CW + 16 + c:O_CW + 16 + c + 1],
                    in1=acc[:, :], op0=ALU.mult, op1=ALU.add),
                    reads=(r_u, r_cst, r_acc), writes=(r_acc,))
                P.add("dve", lambda eng, c=c: eng.tensor_copy(out=ucarry[:, c, :], in_=u[:, TILE:TILE + HALO]),
                      reads=(r_u,), writes=(r_uc,))
                P.add("dve", lambda eng, c=c: eng.tensor_tensor(
                    out=hid[:, c, HALO:HALO + TILE], in0=acc[:, :], in1=gbs[:, HALO:HALO + TILE],
                    op=ALU.mult),
                    reads=(r_acc, r_gb), writes=[rHid(c, 0), rHid(c, 1)])
        out_proj(cwout_d, [(0, SUBS[0]), (1, SUBS[1])], nxt)

    def mlstm_inproj(t, pre=False, hook=None):
        subs = [(0, SUBS[0]), (1, SUBS[1])]
        if pre:
            flush_carry()
        else:
            rmsnorm(O_G + (3 + 1) * 8, subs)
        spill_h(t)
        r_hid_all = [rHid(j, s) for j in range(11) for s in range(2)]
        kc2all = lambda tb: hid[:, tb // 2, (tb % 2) * 512:(tb % 2) * 512 + 512]
        fmst = [hid[:, 4, 0:TILE], hid[:, 5, 0:TILE], hid[:, 8, 0:TILE], hid[:, 9, 0:TILE]]
        vst = [hid[:, 6, 0:2 * DVE_].rearrange("p (a b) -> p a b", a=2),
               hid[:, 7, 0:2 * DVE_].rearrange("p (a b) -> p a b", a=2)]
        r_kc2 = [res("kc2all_%d" % tb) for tb in range(8)]
        r_fm = [res("fmst%d" % i) for i in range(4)]
        s_fm = [P.dmasem("fmst%d" % i) for i in range(4)]
        r_vs = [res("vst0"), res("vst1")]
        s_vs = [P.dmasem("vst0"), P.dmasem("vst1")]
        s_kc = P.dmasem("kc2st")
        r_gq = res("gq")
        s_gq = P.dmasem("gq")
        r_sm = res("sm")
        r_C = [res("C32_%d" % hh) for hh in range(NH)]
        P.add("dve", lambda eng: eng.memset(vst[0][:, :, DV:DVE_], 1.0),
              writes=r_hid_all + r_vs + r_fm + r_kc2)
        P.add("dve", lambda eng: eng.memset(vst[1][:, :, DV:DVE_], 1.0), writes=[r_vs[1]])
        fcnt = [0]

        def fm_proj(c0, ncol, dst, func, hook=None):
            for blk in range(ncol // 512):
                wv, rw, _ = wload([wview(mwin_d, c0 + blk * 512, 512)], KC)
                for mm in range(4):
                    m = blk * 4 + mm
                    b = fcnt[0] % 4
                    fcnt[0] += 1
                    for si, (s0, n) in subs:
                        pp, rpp = mmbank()
                        mm_group(pp, rpp, wv, rw, mm * 128, KC,
                                 lambda k, s0=s0, n=n: xn[:, k, s0:s0 + n],
                                 [rX(k, si) for k in range(KC)], n)
                        P.add("act", lambda eng, pp=pp, n=n, b=b, si=si: eng.activation(
                            out=fmst[b][:, si * SUB:si * SUB + n], in_=pp[:, 0:n], func=func),
                            reads=(rpp,), writes=(r_fm[b],))
                    P.add("sp", lambda eng, b=b, m=m: eng.dma_start(
                        out=dst[t][:, m * TILE:(m + 1) * TILE], in_=fmst[b]),
                        reads=(r_fm[b],), writes=(res("dram_%d" % t),), dma=s_fm[b])
                    if hook is not None:
                        hook(m)

        fm_proj(0, 512, sp_q, AF.Copy)
        fm_proj(512, 512, sp_k, AF.Copy)
        fm_proj(2048, 1024, sp_o, AF.Sigmoid, hook)

        wv_, rw, _ = wload([wview(mwin_d, MIN_DIM - 128, 128)], KC)
        wvg = wv_[:, :, 120:128]
        r_gsc = res("gsc")
        Gv = gsc[:, 0:64].rearrange("p (a b) -> p a b", a=8)
        E1v = gsc[:, 64:96].rearrange("p (a b) -> p a b", a=8)
        SPf = gsc[:, 96:128]
        Tv = gsc[:, 128:160].rearrange("p (a b) -> p a b", a=8)
        pg, rpg = mmbank()

        def f_g(eng, pg=pg):
            out = None
            for tb in range(8):
                tc0 = HALO + tb * 128
                for k in range(KC):
                    out = eng.matmul(pg[:, tb * 8:(tb + 1) * 8], lhsT=xn[:, k, tc0:tc0 + 128], rhs=wvg[:, k, 0:8],
                                     start=(k == 0), stop=(k == KC - 1))
            return out

        P.add("pe", f_g, reads=[rw] + [rX(k, si) for k in range(KC) for si in range(2)], writes=(rpg,))
        P.add("dve", lambda eng: eng.tensor_tensor(
            out=gsc[:, 0:64], in0=pg[:, 0:64], in1=cst[:, O_BG8:O_BG8 + 64], op=ALU.add),
            reads=(rpg, r_cst), writes=(r_gsc,))
        P.add("act", lambda eng: eng.activation(out=E1v, in_=Gv[:, :, 4:8], func=AF.Exp, scale=-1.0),
              reads=(r_gsc,), writes=(r_gsc,))
        P.add("act", lambda eng: eng.activation(out=SPf, in_=gsc[:, 64:96], func=AF.Ln, bias=1.0),
              reads=(r_gsc,), writes=(r_gsc,))
        pb, rpb = mmbank()

        def f_b(eng, pb=pb):
            eng.matmul(pb[:, 0:32], lhsT=tri32, rhs=SPf, start=True, stop=True)
            return eng.matmul(pb[:, 32:64], lhsT=ones32[:, :], rhs=SPf, start=True, stop=True)

        P.add("pe", f_b, reads=(r_gsc, r_cst, r_cbf), writes=(rpb,))
        NB = pb[:, 0:32].rearrange("p (a b) -> p a b", a=8)
        NBL = pb[:, 32:64].rearrange("p (a b) -> p a b", a=8)
        P.add("dve", lambda eng: eng.tensor_tensor(out=Tv, in0=NB, in1=Gv[:, :, 0:4], op=ALU.add),
              reads=(rpb, r_gsc), writes=(r_gsc,))
        P.add("act", lambda eng: eng.activation(out=gq[:, :, 0:4], in_=Tv, func=AF.Exp, bias=LN_KSCALE),
              reads=(r_gsc,), writes=(r_gq,))
        P.add("dve", lambda eng: eng.tensor_tensor(out=Tv, in0=Tv, in1=NBL, op=ALU.subtract),
              reads=(rpb, r_gsc), writes=(r_gsc,))
        P.add("act", lambda eng: eng.activation(out=gq[:, :, 4:8], in_=Tv, func=AF.Exp, bias=LN_KSCALE),
              reads=(r_gsc,), writes=(r_gq,))
        P.add("act", lambda eng: eng.activation(out=gq[:, :, 8:12], in_=NB, func=AF.Exp),
              reads=(rpb,), writes=(r_gq,))
        P.add("act", lambda eng: eng.activation(out=gq[:, :, 12:16], in_=NBL, func=AF.Exp, scale=-1.0),
              reads=(rpb,), writes=(r_gq,))
        P.add("sp", lambda eng: eng.dma_start(out=sp_g[t], in_=gq[:, :, :].rearrange("p a b -> p (a b)")),
              reads=(r_gq,), writes=(res("dram_%d" % t),), dma=s_gq)

        wv, rw, _ = wload([wview(mwin_d, 512, 512)], KC)
        for tb in range(8):
            si = tb // 4
            tc0 = HALO + tb * 128
            pk, rpk = mmbank()

            def f_k(eng, pk=pk, tc0=tc0, wv=wv):
                out = None
                for k in range(KC):
                    out = eng.matmul(pk[:, 0:512], lhsT=xn[:, k, tc0:tc0 + 128], rhs=wv[:, k, 0:512],
                                     start=(k == 0), stop=(k == KC - 1))
                return out

            P.add("pe", f_k, reads=[rw] + [rX(k, si) for k in range(KC)], writes=(rpk,))
            for hh in range(NH):
                if tb % 2 == 0:
                    P.add("dve", lambda eng, pk=pk, tb=tb, hh=hh: eng.tensor_scalar(
                        out=kc2all(tb)[:, hh * 128:(hh + 1) * 128], in0=pk[:, hh * 128:(hh + 1) * 128],
                        scalar1=gq[:, tb, 4 + hh:5 + hh], scalar2=None, op0=ALU.mult),
                        reads=(rpk, r_gq), writes=(r_kc2[tb],))
                else:
                    P.add("act", lambda eng, pk=pk, tb=tb, hh=hh: eng.activation(
                        out=kc2all(tb)[:, hh * 128:(hh + 1) * 128], in_=pk[:, hh * 128:(hh + 1) * 128],
                        func=AF.Copy, scale=gq[:, tb, 4 + hh:5 + hh]),
                        reads=(rpk, r_gq), writes=(r_kc2[tb],))
            P.add("sp", lambda eng, tb=tb: eng.dma_start(out=sp_kc[t * 8 + tb], in_=kc2all(tb)),
                  reads=(r_kc2[tb],), writes=(res("dram_%d" % t),), dma=s_kc)

        vcnt = [0]
        pend = [None]
        for hp in range(2):
            wv, rw, _ = wload([wview(mwin_d, 1024 + hp * 512, 512)], KC)
            for tb in range(8):
                si = tb // 4
                tc0 = HALO + tb * 128
                pv, rpv = mmbank()
                b = vcnt[0] % 2
                vcnt[0] += 1

                def f_v(eng, pv=pv, tc0=tc0, wv=wv):
                    out = None
                    for k in range(KC):
                        out = eng.matmul(pv[:, 0:512], lhsT=xn[:, k, tc0:tc0 + 128], rhs=wv[:, k, 0:512],
                                         start=(k == 0), stop=(k == KC - 1))
                    return out

                P.add("pe", f_v, reads=[rw] + [rX(k, si) for k in range(KC)], writes=(rpv,))
                P.add("act", lambda eng, pv=pv, b=b: eng.activation(
                    out=vst[b][:, :, 0:DV], in_=pv[:, 0:512].rearrange("p (a b) -> p a b", a=2),
                    func=AF.Copy), reads=(rpv,), writes=(r_vs[b],))
                P.add("sp", lambda eng, b=b, tb=tb, hp=hp: eng.dma_start(
                    out=sp_v[t * 8 + tb][:, hp * 2 * DVE_:(hp + 1) * 2 * DVE_],
                    in_=vst[b][:, :, :].rearrange("p a b -> p (a b)")),
                    reads=(r_vs[b],), writes=(res("dram_%d" % t),), dma=s_vs[b])
                def scan(tb=tb, b=b, hp=hp):
                    for hl in range(2):
                        hh = hp * 2 + hl
                        pc, rpc = mmbank()
                        P.add("pe", lambda eng, pc=pc, tb=tb, hh=hh, hl=hl, b=b: eng.matmul(
                            pc[:, 0:DVE_], lhsT=kc2all(tb)[:, hh * 128:(hh + 1) * 128], rhs=vst[b][:, hl, :],
                            start=True, stop=True),
                            reads=(r_kc2[tb], r_vs[b]), writes=(rpc,))
                        P.add("dve", lambda eng, pc=pc, tb=tb, hh=hh: eng.scalar_tensor_tensor(
                            out=C32[:, hh, :], in0=C32[:, hh, :], scalar=gq[:, tb, 12 + hh:13 + hh],
                            in1=pc[:, 0:DVE_], op0=ALU.mult, op1=ALU.add),
                            reads=(rpc, r_gq, r_C[hh]), writes=(r_C[hh],))

                if pend[0] is not None:
                    pend[0]()
                pend[0] = scan
            pend[0]()
            pend[0] = None
        P.add("dve", lambda eng: eng.memset(sm[:, 60:61], 0.0),
              writes=r_kc2 + r_fm + r_vs + r_hid_all)

    def spill_h(t):
        s_h = P.dmasem("sph")
        P.add("sp", lambda eng: eng.dma_start(
            out=sp_h[t].rearrange("p (k n) -> p k n", k=KC), in_=h[:, :, HALO:HALO + TILE]),
            reads=[rH(k, s) for k in range(KC) for s in range(2)], writes=(res("dram_%d" % t),), dma=s_h)


    def mlstm_out(t):
        qT = xn[:, 0:4, HALO:HALO + TILE]
        kT = xn[:, 4:8, HALO:HALO + TILE]
        r_xn_all = [rX(k, s) for k in range(KC) for s in range(2)]
        r_qk = res("qk")
        s_qk = P.dmasem("qk")
        r_gq = res("gq")
        s_gq = P.dmasem("gq")
        r_kcc = [res("kc2c%d" % i) for i in range(3)]
        r_vc = [res("vc%d" % i) for i in range(3)]
        r_so = [res("soc%d" % i) for i in range(3)]
        s_ch = [P.dmasem("chunk%d" % i) for i in range(3)]
        r_C = [res("C32_%d" % hh) for hh in range(NH)]
        r_Cb = [res("Cbf_%d" % hh) for hh in range(NH)]
        r_PT = [res("PT0"), res("PT1")]
        r_hnb = [res("hnb%d" % i) for i in range(4)]
        r_pstc = r_pstb
        r_dram = res("dram_%d" % t)

        def f_qk(eng):
            return [eng.dma_start(out=qT, in_=sp_q[t].rearrange("p (a b) -> p a b", a=NH)),
                    eng.dma_start(out=kT, in_=sp_k[t].rearrange("p (a b) -> p a b", a=NH))]

        P.add("sp", f_qk, reads=(r_dram,), writes=[r_qk] + r_xn_all, dma=s_qk, ndma=2)
        P.add("sp", lambda eng: eng.dma_start(out=gq[:, :, :].rearrange("p a b -> p (a b)"), in_=sp_g[t]),
              reads=(r_dram,), writes=(r_gq,), dma=s_gq)

        def load_chunk(tb):
            b = tb % 3
            tc0 = tb * 128

            def f_ch(eng, b=b, tb=tb, tc0=tc0):
                return [eng.dma_start(out=kc2c[b][:, :], in_=sp_kc[t * 8 + tb]),
                        eng.dma_start(out=vc[b][:, :, :].rearrange("p a b -> p (a b)"), in_=sp_v[t * 8 + tb]),
                        eng.dma_start(out=soc[b][:, :, :],
                                      in_=sp_o[t].rearrange("p (a b) -> p a b", a=KC)[:, :, tc0:tc0 + 128])]

            P.add("sp", f_ch, reads=(r_dram,), writes=(r_kcc[b], r_vc[b], r_so[b]), dma=s_ch[b], ndma=3)

        units = [(tb, hh) for tb in range(8) for hh in range(NH)]
        NU = len(units)
        st = {}
        def stageA(i):
            tb, hh = units[i]
            tc0 = tb * 128
            pS, rpS = psb[i % 2], r_ps[i % 2]
            pb_ = i % 2
            P.add("pe", lambda eng: eng.matmul(
                pS[:, 0:128], lhsT=kT[:, hh, tc0:tc0 + 128], rhs=qT[:, hh, tc0:tc0 + 128],
                start=True, stop=True), reads=(r_qk,), writes=(rpS,))
            P.add("dve", lambda eng: eng.scalar_tensor_tensor(
                out=PT[pb_][:, :], in0=pS[:, 0:128], scalar=gq[:, tb, hh:hh + 1], in1=tri32,
                op0=ALU.mult, op1=ALU.mult),
                reads=(rpS, r_gq, r_cst), writes=(r_PT[pb_],))

        def stageB(i):
            tb, hh = units[i]
            tc0 = tb * 128
            b = tb % 3
            pb_ = i % 2
            pG, rpG = psb[3 + i % 3], r_ps[3 + i % 3]
            pC, rpC = psb[2], r_ps[2]
            hb = i % 4
            o = 24 + (i % 4) * 8
            DN, RD, SS, T1, LT, RS, FF = [sm[:, o + q:o + q + 1] for q in range(7)]
            r_s = res("sm_%d" % (i % 4))

            def f_G(eng):
                eng.matmul(pG[:, 0:DVE_], lhsT=PT[pb_][:, :], rhs=vc[b][:, hh, :], start=True, stop=False)
                return eng.matmul(pG[:, 0:DVE_], lhsT=qT[:, hh, tc0:tc0 + 128], rhs=Cbf[:, hh, :],
                                  start=False, stop=True)

            P.add("pe", f_G, reads=(r_PT[pb_], r_vc[b], r_qk, r_Cb[hh]), writes=(rpG,))
            P.add("pe", lambda eng: eng.matmul(
                pC[:, 0:DVE_], lhsT=kc2c[b][:, hh * 128:(hh + 1) * 128], rhs=vc[b][:, hh, :],
                start=True, stop=True), reads=(r_kcc[b], r_vc[b]), writes=(rpC,))
            P.add("dve", lambda eng: eng.tensor_tensor(
                out=T1, in0=pG[:, DV:DVE_], in1=gq[:, tb, 8 + hh:9 + hh], op=ALU.max),
                reads=(rpG, r_gq), writes=(r_s,))
            P.add("dve", lambda eng: eng.scalar_tensor_tensor(
                out=DN, in0=pG[:, DV:DVE_], scalar=-1.0, in1=T1, op0=ALU.mult, op1=ALU.max),
                reads=(rpG, r_s), writes=(r_s,))
            P.add("dve", lambda eng: eng.reciprocal(out=RD, in_=DN), reads=(r_s,), writes=(r_s,))
            P.add("act", lambda eng: eng.activation(
                out=hnb[hb][:, :], in_=pG[:, 0:DV], func=AF.Square, scale=RD, accum_out=SS),
                reads=(rpG, r_s), writes=(r_s, r_hnb[hb]))
            P.add("dve", lambda eng: eng.scalar_tensor_tensor(
                out=C32[:, hh, :], in0=C32[:, hh, :], scalar=gq[:, tb, 12 + hh:13 + hh],
                in1=pC[:, 0:DVE_], op0=ALU.mult, op1=ALU.add),
                reads=(rpC, r_gq, r_C[hh]), writes=(r_C[hh],))
            P.add("act", lambda eng: eng.activation(
                out=LT, in_=SS, func=AF.Ln, bias=float(EPS), scale=1.0 / DV), reads=(r_s,), writes=(r_s,))
            P.add("act", lambda eng: eng.activation(out=RS, in_=LT, func=AF.Exp, scale=-0.5),
                  reads=(r_s,), writes=(r_s,))
            P.add("dve", lambda eng: eng.tensor_tensor(out=FF, in0=RD, in1=RS, op=ALU.mult),
                  reads=(r_s,), writes=(r_s,))
            P.add("act", lambda eng: eng.activation(
                out=hnb[hb][:, :], in_=pG[:, 0:DV], func=AF.Copy, scale=FF),
                reads=(rpG, r_s), writes=(r_hnb[hb],))
            P.add("act", lambda eng: eng.activation(out=Cbf[:, hh, :], in_=C32[:, hh, :], func=AF.Copy),
                  reads=(r_C[hh],), writes=(r_Cb[hh],))

        def stageC(i):
            tb, hh = units[i]
            tc0 = tb * 128
            si = tb // 4
            b = tb % 3
            hb = i % 4
            pc_ = i % 2
            po = 0
            pst = pstb[pc_]

            def f_T(eng):
                eng.transpose(out=pst[:, po:po + 128], in_=hnb[hb][:, 0:128], identity=ident_bf[:, :])
                return eng.transpose(out=pst[:, po + 128:po + 256], in_=hnb[hb][:, 128:256],
                                     identity=ident_bf[:, :])

            P.add("pe", f_T, reads=(r_hnb[hb], r_cbf), writes=(r_pstc[pc_],))
            for j in range(2):
                c = hh * 2 + j
                P.add("dve", lambda eng, j=j, c=c: eng.scalar_tensor_tensor(
                    out=hid[:, c, HALO + tc0:HALO + tc0 + 128], in0=pst[:, po + j * 128:po + (j + 1) * 128],
                    scalar=cst[:, O_HN + c:O_HN + c + 1], in1=soc[b][:, c, :],
                    op0=ALU.mult, op1=ALU.mult),
                    reads=(r_pstc[pc_], r_cst, r_so[b]), writes=(rHid(c, si),))

        load_chunk(0)
        load_chunk(1)
        load_chunk(2)
        LAGB, LAGC = 1, 3
        for idx in range(NU + LAGC):
            if idx < NU:
                stageA(idx)
            if 0 <= idx - LAGB < NU:
                stageB(idx - LAGB)
            if 0 <= idx - LAGC < NU:
                stageC(idx - LAGC)
                tb_, hh_ = units[idx - LAGC]
                if hh_ == NH - 1 and tb_ + 3 < 8:
                    load_chunk(tb_ + 3)
        P.add("dve", lambda eng: eng.memset(sm[:, 61:62], 0.0), writes=[r_qk] + r_xn_all)

    r_ost = r_xst
    s_ost = s_xst

    def final_out(t, pre=False):
        subs = [(0, SUBS[0]), (1, SUBS[1])]
        if not pre:
            rmsnorm(O_GF, subs, out_bf=False)
        for tb in range(8):
            if tb == 2:
                flush_carry()
            b = xcnt[0] % 4
            xcnt[0] += 1
            si = tb // 4
            c0 = HALO + tb * 128
            for g in range(2):
                pT, rpT = mmbank()

                def f_tr(eng, g=g, c0=c0, pT=pT):
                    out = None
                    for q in range(4):
                        kc = g * 4 + q
                        out = eng.transpose(out=pT[:, q * 128:(q + 1) * 128],
                                            in_=h[:, kc, c0:c0 + 128], identity=ident32)
                    return out

                P.add("pe", f_tr, reads=[rH(g * 4 + q, si) for q in range(4)] + [r_cst], writes=(rpT,))
                if g == 0:
                    P.add("act", lambda eng, b=b, pT=pT: eng.activation(out=xst[b][:, 0:512], in_=pT[:, :], func=AF.Copy),
                          reads=(rpT,), writes=(r_ost[b],))
                else:
                    P.add("dve", lambda eng, b=b, pT=pT: eng.tensor_copy(out=xst[b][:, 512:1024], in_=pT[:, :]),
                          reads=(rpT,), writes=(r_ost[b],))
            row0 = t * TILE + tb * 128
            P.add("sp", lambda eng, b=b, row0=row0: eng.dma_start(out=out_d[row0:row0 + 128, :], in_=xst[b][:, :]),
                  reads=(r_ost[b],), dma=s_ost[b])

    r_Call = [res("C32_%d" % hh) for hh in range(NH)]
    main = [(0, SUBS[0]), (1, SUBS[1])]
    with_halo = [(2, HALO_SUB)] + main
    if do1:
        P.add("dve", lambda eng: eng.memset(C32[:, :, :], 0.0), writes=r_Call)
        for t in range(NT):
            if t == 0:
                load_x(t)
            subs0 = with_halo if t == 0 else main
            G = lambda l_, i_: (O_G + (l_ * 3 + i_) * 8, True)
            if t == 0:
                ffn(0, 0, subs0)
                conv_mixer(t, subs0, nxt=G(0, 2))
            else:
                ffn(0, 0, main, nxt=G(0, 1))
                conv_mixer(t, main, pre=True, nxt=G(0, 2))
            ffn(0, 1, main, pre=True, nxt=G(1, 0))
            ffn(1, 0, main, pre=True, nxt=G(1, 1))
            mlstm_inproj(t, pre=True,
                         hook=(lambda m, t=t: load_x_tb(t + 1, m)) if t + 1 < NT else None)
    s_st = P.dmasem("state")
    if mode == "p1":
        P.add("sp", lambda eng: eng.dma_start(out=st_out[:, :], in_=C32[:, :, :].rearrange("p a b -> p (a b)")),
              reads=r_Call, dma=s_st)
    if mode == "p2":
        P.add("sp", lambda eng: eng.dma_start(out=C32[:, :, :].rearrange("p a b -> p (a b)"), in_=st_in[:, :]),
              writes=r_Call, dma=s_st)
    if mode == "fused":
        r_stl, r_sta = res("st_loc"), res("st_all")
        s_cc = P.dmasem("cc", inc=1)
        s_st2 = P.dmasem("state2")
        P.add("sp", lambda eng: eng.dma_start(out=st_loc[:, :], in_=C32[:, :, :].rearrange("p a b -> p (a b)")),
              reads=r_Call, writes=(r_stl,), dma=s_st)
        P.add("pool", lambda eng: eng.collective_compute(
            "AllGather", ALU.bypass, replica_groups=[[0, 1], [2, 3], [4, 5], [6, 7]],
            ins=[st_loc[:, :]], outs=[st_all[:, :]]),
            reads=(r_stl,), writes=(r_sta,), dma=s_cc)
        P.add("sp", lambda eng: eng.dma_start(out=C32[:, :, :].rearrange("p a b -> p (a b)"), in_=st_all[0:128, :]),
              reads=(r_sta,), writes=r_Call, dma=s_st2)
        P.add("dve", lambda eng: eng.tensor_scalar(
            out=C32[:, :, :], in0=C32[:, :, :], scalar1=cst[:, O_MASK:O_MASK + 1], scalar2=None, op0=ALU.mult),
            reads=r_Call + [r_cst], writes=r_Call)
    if do2:
        r_Cb = [res("Cbf_%d" % hh) for hh in range(NH)]
        for hh in range(NH):
            P.add("act", lambda eng, hh=hh: eng.activation(out=Cbf[:, hh, :], in_=C32[:, hh, :], func=AF.Copy),
                  reads=(r_Call[hh],), writes=(r_Cb[hh],))
        for b in range(3):
            P.add("dve", lambda eng, b=b: eng.memset(vc[b][:, :, :], 1.0), writes=(res("vc%d" % b),))
        s_h = P.dmasem("sph")
        for t in range(NT):
            P.add("sp", lambda eng, t=t: eng.dma_start(
                out=h[:, :, HALO:HALO + TILE], in_=sp_h[t].rearrange("p (k n) -> p k n", k=KC)),
                reads=(res("dram_%d" % t),),
                writes=[rH(k, s) for k in range(KC) for s in range(2)], dma=s_h)
            mlstm_out(t)
            out_proj(mwout_d, main, nxt=(O_G + 5 * 8, True))
            ffn(1, 1, main, pre=True, nxt=(O_GF, False))
            final_out(t, pre=True)
            flush_carry()
    allres = list(R.values())
    P.add("sp", lambda eng: eng.nop(), reads=allres, writes=allres)
    P.emit(nc, es)
    es.close()
    return nc


def _consts(norm_g, final_norm_g, conv_w, head_norm, b_gates):
    c = np.zeros((128, NCONST), np.float32)
    c[:, O_ID:O_ID + 128] = np.eye(128, dtype=np.float32)
    c[:, O_TRI:O_TRI + 128] = np.triu(np.ones((128, 128), np.float32))
    c[:, O_G:O_G + 48] = norm_g.reshape(6, KC, 128).transpose(2, 0, 1).reshape(128, 48)
    c[:, O_GF:O_GF + 8] = final_norm_g.reshape(KC, 128).T
    c[:, O_CW:O_CW + 24] = conv_w.reshape(3, KC, 128).transpose(2, 0, 1).reshape(128, 24)
    c[:, O_HN:O_HN + 8] = head_norm.reshape(KC, 128).T
    c[:, O_BG:O_BG + 8] = np.broadcast_to(b_gates.reshape(1, 8), (128, 8))
    c[:, O_BG8:O_BG8 + 64] = np.tile(b_gates.reshape(1, 8), (128, 8))
    return c


_NC_CACHE = {}


def _get_nc(mode):
    if mode not in _NC_CACHE:
        _NC_CACHE[mode] = build(mode)
    return _NC_CACHE[mode]


MODE = "fused"


def kernel(x, norm_g, ffn_w_gate, ffn_w_up, ffn_w_down, conv_w_in, conv_w, conv_w_out,
           mlstm_w_in, mlstm_b_gates, mlstm_head_norm, mlstm_w_out, final_norm_g):
    f32 = lambda a: np.ascontiguousarray(np.asarray(a, dtype=np.float32))
    x = f32(x)
    consts = _consts(f32(norm_g), f32(final_norm_g), f32(conv_w[0]), f32(mlstm_head_norm[0]),
                     f32(mlstm_b_gates[0]))
    wg, wu, wd = f32(ffn_w_gate), f32(ffn_w_up), f32(ffn_w_down)
    xs = []
    for c in range(NCORES):
        b, half = c // 2, c % 2
        xc = np.zeros((TOK + HALO, D), np.float32)
        xc[HALO:] = x[b, half * TOK:(half + 1) * TOK]
        if half == 1:
            xc[:HALO] = x[b, TOK - HALO:TOK]
        xs.append(xc)
    common = {"ffn_w_gate": wg, "ffn_w_up": wu, "ffn_w_down": wd}
    cs = []
    for c in range(NCORES):
        cc_ = consts.copy()
        cc_[:, O_MASK] = float(c % 2)
        cs.append(cc_)
    p1_w = {"conv_w_in": f32(conv_w_in[0]), "conv_w_out": f32(conv_w_out[0]), "mlstm_w_in": f32(mlstm_w_in[0])}
    p2_w = {"mlstm_w_out": f32(mlstm_w_out[0])}
    cores = list(range(NCORES))
    if MODE == "two":
        nc1 = _get_nc("p1")
        r1 = run_bass_kernel_spmd(nc1, [dict(common, **p1_w, x=xs[c], consts=cs[c]) for c in cores], core_ids=cores).results
        nc2 = _get_nc("p2")
        in2 = []
        for c in cores:
            d = dict(common, **p2_w, consts=cs[c])
            for k in ("sp_h", "sp_q", "sp_k", "sp_o", "sp_kc", "sp_v", "sp_g"):
                d[k] = r1[c][k]
            d["st_in"] = (np.zeros((128, NH * DVE_), np.float32) if c % 2 == 0
                          else np.ascontiguousarray(r1[c - 1]["st_out"]))
            in2.append(d)
        r2 = run_bass_kernel_spmd(nc2, in2, core_ids=cores).results
        outs = [r2[c]["out"] for c in cores]
    else:
        ncf = _get_nc("fused")
        r = run_bass_kernel_spmd(ncf, [dict(common, **p1_w, **p2_w, x=xs[c], consts=cs[c]) for c in cores],
                                 core_ids=cores).results
        outs = [r[c]["out"] for c in cores]
    out = np.zeros((4, SEQ, D), np.float32)
    for c in cores:
        b, half = c // 2, c % 2
        out[b, half * TOK:(half + 1) * TOK] = np.asarray(outs[c], dtype=np.float32).reshape(TOK, D)
    return out
```
